# Optimizing a Trainium2 kernel written in Bass

```python
import math
import jax, jax.numpy as jnp
from jax import lax
import numpy as np

D_MODEL = 1024
BATCH = 2
SEQ = 16384
DEPTH = 1

HEAD_DIM = 64
DIL_WIDTH = D_MODEL // 2
N_HEADS_DIL = DIL_WIDTH // HEAD_DIM
DIL_PATTERNS = ((128, 1), (512, 4), (2048, 16))
DIFF_WIDTH = D_MODEL - DIL_WIDTH
N_HEADS_DIFF = DIFF_WIDTH // (2 * HEAD_DIM)
MIX_WIDTH = DIL_WIDTH + DIFF_WIDTH
Q_BLOCK = 128
N_EXPERTS = 32
TOP_K = 4
D_FF = D_MODEL
SWIGLU_ALPHA = 1.702
SWIGLU_LIMIT = 7.0
MOE_BLOCK = 256
DEEPNORM_ALPHA = (2.0 * DEPTH) ** 0.25
DEEPNORM_BETA = (8.0 * DEPTH) ** -0.25
LN_EPS = 1e-5
NEG_INF = -1e30

kernel_name = "hybrid_dilated_diffattn_moe_deepnorm"


def alibi_slopes(n):
    return jnp.asarray([2.0 ** (-8.0 * (i + 1) / n) for i in range(n)], jnp.float32)


def layer_norm(x, g, b):
    xf = x.astype(jnp.float32)
    mu = xf.mean(-1, keepdims=True)
    var = jnp.square(xf - mu).mean(-1, keepdims=True)
    return ((xf - mu) * lax.rsqrt(var + LN_EPS) * g + b).astype(x.dtype)


def dilated_pattern(q, k, v, window, dilation, slopes):
    B, H, S, Dh = q.shape
    n_side = (window // 2) // dilation
    L = S // dilation
    blk = n_side
    nb = -(-L // blk)
    Lp = nb * blk
    span = blk + 2 * n_side

    def to_res(a):
        return a.reshape(B, H, L, dilation, Dh).transpose(0, 1, 3, 2, 4)

    qr = jnp.pad(to_res(q), ((0, 0), (0, 0), (0, 0), (0, Lp - L), (0, 0)))
    kr = jnp.pad(to_res(k), ((0, 0), (0, 0), (0, 0), (n_side, n_side + Lp - L), (0, 0)))
    vr = jnp.pad(to_res(v), ((0, 0), (0, 0), (0, 0), (n_side, n_side + Lp - L), (0, 0)))
    kidx = (jnp.arange(nb) * blk)[:, None] + jnp.arange(span)[None, :]
    kb = kr[:, :, :, kidx]
    vb = vr[:, :, :, kidx].astype(jnp.float32)
    qb = qr.reshape(B, H, dilation, nb, blk, Dh)
    s = jnp.einsum('bhrnqd,bhrnkd->bhrnqk', qb, kb,
                   preferred_element_type=jnp.float32) * (HEAD_DIM ** -0.5)
    rel = jnp.arange(span)[None, :] - n_side - jnp.arange(blk)[:, None]
    key_m = kidx - n_side
    valid = (jnp.abs(rel) <= n_side)[None] & ((key_m >= 0) & (key_m < L))[:, None, :]
    bias = -(slopes * dilation)[:, None, None] * jnp.abs(rel).astype(jnp.float32)
    s = jnp.where(valid[None, None, None], s + bias[None, :, None, None], NEG_INF)
    m = s.max(-1)
    p = jnp.exp(s - m[..., None])
    l = p.sum(-1)
    acc = jnp.einsum('bhrnqk,bhrnkd->bhrnqd', p, vb)

    def from_res(a):
        tail = a.shape[5:]
        a = a.reshape(B, H, dilation, Lp, *tail)[:, :, :, :L]
        a = jnp.moveaxis(a, 2, 3)
        return a.reshape(B, H, S, *tail)

    return from_res(m), from_res(l), from_res(acc)


def dilated_attention(q, k, v, slopes):
    results = [dilated_pattern(q, k, v, w, d, slopes) for (w, d) in DIL_PATTERNS]
    ms = jnp.stack([r[0] for r in results])
    ls = jnp.stack([r[1] for r in results])
    accs = jnp.stack([r[2] for r in results])
    wts = jnp.exp(ms - ms.max(0, keepdims=True))
    num = (wts[..., None] * accs).sum(0)
    den = (wts * ls).sum(0)
    return num / den[..., None]


def diff_attention(q, k, v, slopes, lam):
    B, H, _, S, Dh = q.shape
    nq = S // Q_BLOCK
    qb = q.reshape(B, H, 2, nq, Q_BLOCK, Dh).transpose(3, 0, 1, 2, 4, 5)
    vf = v.astype(jnp.float32)
    key_pos = jnp.arange(S)

    def block(args):
        qblk, i = args
        s = jnp.einsum('bhcqd,bhckd->bhcqk', qblk, k,
                       preferred_element_type=jnp.float32) * (HEAD_DIM ** -0.5)
        qpos = i * Q_BLOCK + jnp.arange(Q_BLOCK)
        dist = jnp.abs(qpos[:, None] - key_pos[None, :]).astype(jnp.float32)
        s = s - (slopes[:, None, None] * dist)[None, :, None]
        p = jax.nn.softmax(s, axis=-1)
        a = p[:, :, 0] - lam * p[:, :, 1]
        return jnp.einsum('bhqk,bhkd->bhqd', a, vf)

    o = lax.map(block, (qb, jnp.arange(nq)))
    return o.transpose(1, 2, 0, 3, 4).reshape(B, H, S, 2 * Dh)


def token_mixers(h, w_in, w_out, lq1, lk1, lq2, lk2, g_sub, layer_idx):
    B, S, _ = h.shape
    proj = h @ w_in
    cuts = np.cumsum([DIL_WIDTH, DIL_WIDTH, DIL_WIDTH, DIFF_WIDTH, DIFF_WIDTH])
    qa, ka, va, qd, kd, vd = jnp.split(proj, [int(c) for c in cuts], axis=-1)

    def heads_a(t):
        return t.reshape(B, S, N_HEADS_DIL, HEAD_DIM).transpose(0, 2, 1, 3)

    oa = dilated_attention(heads_a(qa), heads_a(ka), heads_a(va), alibi_slopes(N_HEADS_DIL))
    oa = oa.transpose(0, 2, 1, 3).reshape(B, S, DIL_WIDTH)

    def heads_qk(t):
        return t.reshape(B, S, N_HEADS_DIFF, 2, HEAD_DIM).transpose(0, 2, 3, 1, 4)

    lam_init = 0.8 - 0.6 * math.exp(-0.3 * layer_idx)
    lam = (jnp.exp(jnp.sum(lq1.astype(jnp.float32) * lk1.astype(jnp.float32)))
           - jnp.exp(jnp.sum(lq2.astype(jnp.float32) * lk2.astype(jnp.float32))) + lam_init)
    vdh = vd.reshape(B, S, N_HEADS_DIFF, 2 * HEAD_DIM).transpose(0, 2, 1, 3)
    od = diff_attention(heads_qk(qd), heads_qk(kd), vdh, alibi_slopes(N_HEADS_DIFF), lam)
    od = od * lax.rsqrt(jnp.mean(od * od, -1, keepdims=True) + LN_EPS) * g_sub.astype(jnp.float32)
    od = (od * (1.0 - lam_init)).transpose(0, 2, 1, 3).reshape(B, S, DIFF_WIDTH)

    mixed = jnp.concatenate([oa, od], axis=-1).astype(h.dtype)
    return mixed @ w_out


def moe(h, w_router, b_router, w_up, b_up, w_down, b_down):
    B, S, D = h.shape
    T = B * S
    xf = h.reshape(T, D)
    logits = (xf @ w_router).astype(jnp.float32) + b_router.astype(jnp.float32)
    top_vals, top_idx = lax.top_k(logits, TOP_K)
    gates = jax.nn.softmax(top_vals, axis=-1)

    TK = T * TOP_K
    flat_e = top_idx.reshape(-1).astype(jnp.int32)
    flat_tok = jnp.arange(TK, dtype=jnp.int32) // TOP_K
    flat_gate = gates.reshape(-1)
    order = jnp.argsort(flat_e, stable=True)
    sorted_e = flat_e[order]
    counts = jnp.bincount(flat_e, length=N_EXPERTS)
    starts = jnp.cumsum(counts) - counts
    pcounts = ((counts + MOE_BLOCK - 1) // MOE_BLOCK) * MOE_BLOCK
    pends = jnp.cumsum(pcounts)
    pstarts = pends - pcounts
    dest = pstarts[sorted_e] + (jnp.arange(TK) - starts[sorted_e])

    n_rows = (-(-TK // MOE_BLOCK)) * MOE_BLOCK + N_EXPERTS * MOE_BLOCK
    nb = n_rows // MOE_BLOCK
    row_tok = jnp.full((n_rows,), T, jnp.int32).at[dest].set(flat_tok[order])
    row_gate = jnp.zeros((n_rows,), jnp.float32).at[dest].set(flat_gate[order])
    block_e = jnp.clip(jnp.searchsorted(pends, jnp.arange(nb) * MOE_BLOCK, side='right'),
                       0, N_EXPERTS - 1)
    x_pad = jnp.concatenate([xf, jnp.zeros((1, D), xf.dtype)], axis=0)

    def expert_block(args):
        tok, e = args
        hu = x_pad[tok] @ w_up[e] + b_up[e]
        g, u = hu[:, :D_FF], hu[:, D_FF:]
        g = jnp.minimum(g, SWIGLU_LIMIT)
        u = jnp.clip(u, -SWIGLU_LIMIT, SWIGLU_LIMIT)
        act = g * jax.nn.sigmoid(SWIGLU_ALPHA * g) * (u + 1.0)
        return act @ w_down[e] + b_down[e]

    y = lax.map(expert_block, (row_tok.reshape(nb, MOE_BLOCK), block_e)).reshape(n_rows, D)
    out = jnp.zeros((T + 1, D), jnp.float32).at[row_tok].add(y.astype(jnp.float32) * row_gate[:, None])
    return out[:T].reshape(B, S, D).astype(h.dtype)


def setup_inputs(seed: int = 0) -> dict:
    key = jax.random.key(seed)
    ks = jax.random.split(key, 20)
    n = lambda k, shape: jax.random.normal(k, shape, jnp.float32)
    return {
        "x": n(ks[0], (BATCH, SEQ, D_MODEL)),
        "w_in": n(ks[1], (DEPTH, D_MODEL, 3 * MIX_WIDTH)) * D_MODEL ** -0.5,
        "w_out": n(ks[2], (DEPTH, MIX_WIDTH, D_MODEL)) * MIX_WIDTH ** -0.5 * DEEPNORM_BETA,
        "lambda_q1": n(ks[3], (DEPTH, HEAD_DIM)) * 0.1,
        "lambda_k1": n(ks[4], (DEPTH, HEAD_DIM)) * 0.1,
        "lambda_q2": n(ks[5], (DEPTH, HEAD_DIM)) * 0.1,
        "lambda_k2": n(ks[6], (DEPTH, HEAD_DIM)) * 0.1,
        "diff_norm_g": 1.0 + 0.02 * n(ks[7], (DEPTH, 2 * HEAD_DIM)),
        "ln1_g": 1.0 + 0.02 * n(ks[8], (DEPTH, D_MODEL)),
        "ln1_b": 0.02 * n(ks[9], (DEPTH, D_MODEL)),
        "w_router": n(ks[10], (DEPTH, D_MODEL, N_EXPERTS)) * D_MODEL ** -0.5,
        "b_router": 0.01 * n(ks[11], (DEPTH, N_EXPERTS)),
        "w_up": n(ks[12], (DEPTH, N_EXPERTS, D_MODEL, 2 * D_FF)) * D_MODEL ** -0.5,
        "b_up": 0.01 * n(ks[13], (DEPTH, N_EXPERTS, 2 * D_FF)),
        "w_down": n(ks[14], (DEPTH, N_EXPERTS, D_FF, D_MODEL)) * D_FF ** -0.5 * DEEPNORM_BETA,
        "b_down": 0.01 * n(ks[15], (DEPTH, N_EXPERTS, D_MODEL)),
        "ln2_g": 1.0 + 0.02 * n(ks[16], (DEPTH, D_MODEL)),
        "ln2_b": 0.02 * n(ks[17], (DEPTH, D_MODEL)),
    }


def reference(x, w_in, w_out, lambda_q1, lambda_k1, lambda_q2, lambda_k2, diff_norm_g,
              ln1_g, ln1_b, w_router, b_router, w_up, b_up, w_down, b_down, ln2_g, ln2_b):
    for l in range(DEPTH):
        mix = token_mixers(x, w_in[l], w_out[l], lambda_q1[l], lambda_k1[l], lambda_q2[l],
                           lambda_k2[l], diff_norm_g[l], l)
        x = layer_norm(DEEPNORM_ALPHA * x + mix, ln1_g[l], ln1_b[l])
        ffn = moe(x, w_router[l], b_router[l], w_up[l], b_up[l], w_down[l], b_down[l])
        x = layer_norm(DEEPNORM_ALPHA * x + ffn, ln2_g[l], ln2_b[l])
    return x
```

```python
import math
import numpy as np
import concourse.bass as bass
import concourse.mybir as mybir
from concourse.bass_utils import run_bass_kernel_spmd
from concourse.alu_op_type import AluOpType as ALU

F32, BF16, I32 = mybir.dt.float32, mybir.dt.bfloat16, mybir.dt.int32
AF = mybir.ActivationFunctionType

NCORES = 8
S = 16384
D = 1024
OWN = 4096
NT = OWN // 128
CAP = 768
NE = 32
ALPHA = 2.0 ** 0.25
EPS = 1e-5
LAM_INIT = 0.8 - 0.6 * math.exp(0.0)
NEG = -30000.0
LA = (4224, 4608, 6144)
LA_OFF = (0, 4224, 8832)
LAT = sum(LA)
DIL = (1, 4, 16)

ENGS = ("pe", "act", "dve", "pool", "sp")


class Op:
    __slots__ = ("eng", "emit", "raw", "war", "signal", "val", "semkey", "dma", "waits", "idx")


class Prog:
    def __init__(self):
        self.ops = []
        self.lastw = {}
        self.readers = {}
        self.phase = 0
        self.fence = None
        self.last_by_key = {}

    def add(self, eng, emit, reads=(), writes=(), chan=None):
        op = Op()
        op.eng, op.emit, op.dma = eng, emit, chan
        op.semkey = ("dma", chan) if chan is not None else (eng, self.phase)
        op.signal = chan is not None
        op.val = 0
        op.idx = len(self.ops)
        raw, war = set(), set()
        for r in reads:
            raw.update(self.lastw.get(r, {}).values())
        for r in writes:
            if not r[0].isupper():
                raw.update(self.lastw.get(r, {}).values())
            war.update(self.readers.get(r, {}).values())
        if self.fence is not None:
            raw.add(self.fence)
        for r in reads:
            self.readers.setdefault(r, {})[op.semkey] = op
        for r in writes:
            if r[0].isupper():
                self.lastw.setdefault(r, {})[op.semkey] = op
            else:
                self.lastw[r] = {op.semkey: op}
            self.readers[r] = {}
        op.raw, op.war = raw, war
        self.ops.append(op)
        self.last_by_key[op.semkey] = op
        return op

    def barrier(self):
        deps = list(self.last_by_key.values())
        op = self.add("dve", None)
        op.raw = set(d for d in deps if d is not op)
        if self.fence is not None:
            op.raw.add(self.fence)
        self.fence = op
        self.phase += 1
        return op

    def finalize(self):
        for op in self.ops:
            best = {}
            for x, is_raw in [(x, True) for x in op.raw] + [(x, False) for x in op.war]:
                if x is op:
                    continue
                if x.dma is None and x.eng == op.eng and op.dma is None:
                    if op.eng == "pe":
                        continue
                    if not is_raw:
                        continue
                if x.dma is not None and op.dma is not None and x.semkey == op.semkey:
                    continue
                k = x.semkey
                if k not in best or best[k].idx < x.idx:
                    best[k] = x
            op.waits = list(best.values())
            for x in op.waits:
                x.signal = True
        counters = {}
        for op in self.ops:
            if op.signal:
                inc = 16 if op.dma is not None else 1
                counters[op.semkey] = counters.get(op.semkey, 0) + inc
                op.val = counters[op.semkey]
        return counters

    def prepare(self, nc):
        counters = self.finalize()
        self.sems = {k: nc.alloc_semaphore("s_%s_%s" % (str(k[0]), str(k[1]))) for k in counters}

    def emit_engine(self, e, eng):
        sems = self.sems
        wd = {}
        for op in self.ops:
            if op.eng != e:
                continue
            for x in op.waits:
                if wd.get(x.semkey, 0) >= x.val:
                    continue
                eng.wait_ge(sems[x.semkey], x.val)
                wd[x.semkey] = x.val
            ins = None
            if op.emit is not None:
                ins = op.emit(eng)
            if op.signal:
                if ins is None:
                    ins = eng.nop()
                ins.then_inc(sems[op.semkey], 16 if op.dma is not None else 1)


def build_program(debug=False):
    nc = bass.Bass("TRN2", target_bir_lowering=False)
    P = Prog()

    def din(name, shape, dt=F32):
        return nc.dram_tensor(name, list(shape), dt, kind="ExternalInput").ap()

    def dscr(name, shape, dt):
        return nc.dram_tensor(name, list(shape), dt, kind=("ExternalOutput" if debug else "Internal")).ap()

    xT_all = din("xT_all", [D, S])
    xTa = din("xTa", [D, LAT])
    x_own = din("x_own", [OWN, D])
    w_in = din("w_in", [D, 3072])
    w_out = din("w_out", [D, D])
    w_router = din("w_router", [D, NE])
    b_router = din("b_router", [1, NE])
    w_up = din("w_up", [NE, D, 2048])
    b_upT = din("b_upT", [128, NE * 16])
    w_down = din("w_down", [NE, D, D])
    b_down = din("b_down", [NE, D])
    ln1_g = din("ln1_g", [1, D]); ln1_b = din("ln1_b", [1, D])
    ln2_g = din("ln2_g", [1, D]); ln2_b = din("ln2_b", [1, D])
    lam_in = din("lam_in", [1, 256])
    gsub = din("gsub", [1, 128])
    kvalid = din("kvalid", [1, LAT])
    kaugB = din("kaugB", [4, S])
    qaugB = din("qaugB", [4, 8, 4, 3 * 2 * 512])
    diagB_in = din("diagB", [128, 4 * 128])
    biasA_in = din("biasA", [128, 12 * 512])
    consts = din("consts", [128, 5 * 128])
    out_d = nc.dram_tensor("out", [OWN, D], F32, kind="ExternalOutput").ap()

    KT_B = dscr("KT_B", [4, 128, S], BF16)
    V_B = dscr("V_B", [S, 512], BF16)
    QT_B = dscr("QT_B", [4, 128, OWN], BF16)
    QT_A = dscr("QT_A", [512, LAT], BF16)
    KT_A = dscr("KT_A", [512, LAT], BF16)
    V_A = dscr("V_A", [LAT, 512], BF16)
    MIXT = dscr("MIXT", [D, OWN], BF16)
    X1 = dscr("X1", [OWN, D], F32)
    XE = dscr("XE", [NE * CAP, D], BF16)
    YS = dscr("YS", [NE * CAP, D], F32)

    ARENA_BYTES = 188 * 1024
    arena = nc.alloc_sbuf_tensor("arena", [128, ARENA_BYTES // 2], BF16)
    ps = nc.alloc_psum_tensor("ps", [128, 8, 512], F32)

    def view(off, shape, dt, p0=0, p1=128):
        nbytes = int(np.prod(shape)) * (4 if dt in (F32, I32) else 2)
        assert off % 32 == 0 and off + nbytes <= ARENA_BYTES, (off, nbytes)
        a = arena[p0:p1, off // 2: (off + nbytes) // 2]
        if dt != BF16:
            a = a.bitcast(dt)
        if len(shape) == 2:
            return a.rearrange("p (a b) -> p a b", b=shape[1])
        if len(shape) == 3:
            return a.rearrange("p (a b c) -> p a b c", b=shape[1], c=shape[2])
        return a

    def small(name, shape, dt=F32):
        return nc.alloc_sbuf_tensor(name, [128] + list(shape), dt)

    ident_f = small("ident_f", [128]); ltri_f = small("ltri_f", [128]); ones_f = small("ones_f", [128])
    ident_b = small("ident_b", [128], BF16); ltri_b = small("ltri_b", [128], BF16); ones_b = small("ones_b", [128], BF16)
    eoff = small("eoff", [32])
    lamv = small("lamv", [256]); lamp = small("lamp", [128]); lam2 = small("lam2", [4])
    nlam = small("nlam", [1])
    gsc = small("gsc", [128])
    brt = small("brt", [32])
    sl_f = small("sl_f", [NT * 4]); sl_i = small("sl_i", [NT * 4], I32); gatek = small("gatek", [NT * 4])
    cum_f = small("cum_f", [32]); cum_b = small("cum_b", [32], BF16)
    scr1 = small("scr1", [8])

    def dma(eng, chan, out, in_, reads=(), writes=()):
        if eng == "pool":
            return P.add(eng, lambda e: e.dma_start(out=out, in_=in_, max_dma_last_dim=8192), reads, writes, chan=chan)
        return P.add(eng, lambda e: e.dma_start(out=out, in_=in_), reads, writes, chan=chan)

    def pe(fn, reads=(), writes=()):
        return P.add("pe", fn, reads, writes)

    def act(fn, reads=(), writes=()):
        return P.add("act", fn, reads, writes)

    def dve(fn, reads=(), writes=()):
        return P.add("dve", fn, reads, writes)

    dma("sp", "c0", ident_f[:, :], consts[:, 0:128], (), ("ident_f",))
    dma("sp", "c1", ltri_f[:, :], consts[:, 128:256], (), ("ltri_f",))
    dma("sp", "c2", ones_f[:, :], consts[:, 256:384], (), ("ones_f",))
    dma("sp", "c3", eoff[:, :], consts[:, 384:416], (), ("eoff",))
    dma("sp", "c4", lamv[:, :], lam_in.broadcast_to([128, 256]), (), ("lamv",))
    dma("sp", "c5", gsc[:, :], gsub.broadcast_to([128, 128]), (), ("gsc",))
    dma("sp", "c6", brt[:, :], b_router.broadcast_to([128, NE]), (), ("brt",))
    dve(lambda e: e.tensor_copy(out=ident_b[:, :], in_=ident_f[:, :]), ("ident_f",), ("ident_b",))
    dve(lambda e: e.tensor_copy(out=ltri_b[:, :], in_=ltri_f[:, :]), ("ltri_f",), ("ltri_b",))
    dve(lambda e: e.tensor_copy(out=ones_b[:, :], in_=ones_f[:, :]), ("ones_f",), ("ones_b",))
    dve(lambda e: e.memset(cum_f[:, :], 0.0), (), ("cum_f",))
    dve(lambda e: e.memset(cum_b[:, :], 0.0), (), ("cum_b",))
    dve(lambda e: e.tensor_tensor(out=lamp[:, 0:64], in0=lamv[:, 0:64], in1=lamv[:, 64:128], op=ALU.mult), ("lamv",), ("lamp",))
    dve(lambda e: e.tensor_tensor(out=lamp[:, 64:128], in0=lamv[:, 128:192], in1=lamv[:, 192:256], op=ALU.mult), ("lamv", "lamp"), ("lamp",))
    dve(lambda e: e.reduce_sum(out=lam2[:, 0:1], in_=lamp[:, 0:64], axis=mybir.AxisListType.X), ("lamp",), ("lam2a",))
    dve(lambda e: e.reduce_sum(out=lam2[:, 1:2], in_=lamp[:, 64:128], axis=mybir.AxisListType.X), ("lamp",), ("lam2b",))
    act(lambda e: e.activation(out=lam2[:, 2:4], in_=lam2[:, 0:2], func=AF.Exp), ("lam2a", "lam2b"), ("lam2c",))
    dve(lambda e: e.tensor_tensor(out=nlam[:, :], in0=lam2[:, 3:4], in1=lam2[:, 2:3], op=ALU.subtract), ("lam2c",), ("nlam",))
    dve(lambda e: e.tensor_scalar(out=nlam[:, :], in0=nlam[:, :], scalar1=-LAM_INIT, scalar2=None, op0=ALU.add), ("nlam",), ("nlam",))
    dve(lambda e: e.tensor_scalar(out=gsc[:, :], in0=gsc[:, :], scalar1=1.0 - LAM_INIT, scalar2=None, op0=ALU.mult), ("gsc",), ("gsc",))
    P.barrier()

    win = view(0, [8, 3072], BF16)
    SUP = 2048
    xts = [view(49152 + 32768 * i, [8, SUP], BF16) for i in range(2)]
    NSTG = 6
    stgs = [view(114688 + 1024 * i, [1, 512], BF16) for i in range(NSTG)]
    for c in range(8):
        dma("pool", "win", win[:, c, :], w_in[c * 128:(c + 1) * 128, :], (), ("win",))
    cnt = {"chunk": 0, "grp": 0}

    def proj_stream(src, L, outs):
        for u0 in range(0, L, SUP):
            TS = min(SUP, L - u0)
            ci = cnt["chunk"]; cnt["chunk"] += 1
            xt = xts[ci % 2]
            xr = "xt%d" % (ci % 2)
            for c in range(8):
                dma("pool", xr, xt[:, c, 0:TS], src[c * 128:(c + 1) * 128, u0:u0 + TS], (), (xr,))
            for s0 in range(0, TS, 512):
                t0 = u0 + s0
                T = min(512, TS - s0)
                for o in outs:
                    if t0 >= o["tmax"]:
                        continue
                    if o["kind"] == "fm":
                        for ft in range(o["nf"] // 128):
                            g = cnt["grp"]; cnt["grp"] += 1
                            bank = g % 8
                            pb = "ps%d" % bank
                            st = stgs[g % NSTG]; sr = "stg%d" % (g % NSTG)
                            f0 = o["w0"] + ft * 128

                            def mm(e, xt=xt, bank=bank, f0=f0, T=T, s0=s0):
                                for c in range(8):
                                    i = e.matmul(ps[:, bank, 0:T], lhsT=win[:, c, f0:f0 + 128], rhs=xt[:, c, s0:s0 + T],
                                                 start=(c == 0), stop=(c == 7))
                                return i
                            pe(mm, (xr, "win"), (pb,))
                            sc = o.get("scale", 1.0)
                            if g % 2 == 0:
                                dve(lambda e, st=st, bank=bank, T=T, sc=sc: e.tensor_scalar(
                                    out=st[:, 0, 0:T], in0=ps[:, bank, 0:T], scalar1=sc, scalar2=None, op0=ALU.mult), (pb,), (sr,))
                            else:
                                act(lambda e, st=st, bank=bank, T=T, sc=sc: e.activation(
                                    out=st[:, 0, 0:T], in_=ps[:, bank, 0:T], func=AF.Copy, scale=sc), (pb,), (sr,))
                            dma("sp", sr, o["dst"](ft, t0, T), st[:, 0, 0:T], (sr,), (o["res"],))
                    else:
                        for sub in range(T // 128):
                            g = cnt["grp"]; cnt["grp"] += 1
                            bank = g % 8
                            pb = "ps%d" % bank
                            st = stgs[g % NSTG]; sr = "stg%d" % (g % NSTG)

                            def mm(e, xt=xt, bank=bank, sub=sub, w0=o["w0"], s0=s0):
                                for c in range(8):
                                    i = e.matmul(ps[:, bank, :], lhsT=xt[:, c, s0 + sub * 128:s0 + (sub + 1) * 128],
                                                 rhs=win[:, c, w0:w0 + 512], start=(c == 0), stop=(c == 7))
                                return i
                            pe(mm, (xr, "win"), (pb,))
                            if g % 2 == 0:
                                dve(lambda e, st=st, bank=bank: e.tensor_copy(out=st[:, 0, :], in_=ps[:, bank, :]), (pb,), (sr,))
                            else:
                                act(lambda e, st=st, bank=bank: e.activation(out=st[:, 0, :], in_=ps[:, bank, :], func=AF.Copy), (pb,), (sr,))
                            dma("sp", sr, o["dst"](t0 + sub * 128), st[:, 0, :], (sr,), (o["res"],))

    proj_stream(xT_all, S, [
        dict(kind="fm", w0=2048, nf=512, tmax=S, res="KT_B", dst=lambda ft, t0, T: KT_B[ft, :, t0:t0 + T]),
        dict(kind="tm", w0=2560, tmax=S, res="V_B", dst=lambda tok: V_B[tok:tok + 128, :]),
        dict(kind="fm", w0=1536, nf=512, tmax=OWN, scale=0.125, res="QT_B", dst=lambda ft, t0, T: QT_B[ft, :, t0:t0 + T]),
    ])
    proj_stream(xTa, LAT, [
        dict(kind="fm", w0=0, nf=512, tmax=LAT, scale=0.125, res="QT_A", dst=lambda ft, t0, T: QT_A[ft * 128:(ft + 1) * 128, t0:t0 + T]),
        dict(kind="fm", w0=512, nf=512, tmax=LAT, res="KT_A", dst=lambda ft, t0, T: KT_A[ft * 128:(ft + 1) * 128, t0:t0 + T]),
        dict(kind="tm", w0=1024, tmax=LAT, res="V_A", dst=lambda tok: V_A[tok:tok + 128, :]),
    ])
    P.barrier()

    biasA = view(0, [12, 512], BF16)
    qas = [view(12288 + 12288 * i, [1, 6144], BF16) for i in range(2)]
    kas = [view(36864 + 12288 * i, [1, 6144], BF16) for i in range(2)]
    vas = [view(61440 + 6272 * i, [48, 65], BF16) for i in range(2)]
    accO = view(73984, [1, 4096], F32)
    pTa = [view(90368 + 1024 * i, [1, 512], BF16) for i in range(2)]
    rl = view(92416, [1, 4096], F32)
    mst = [view(108800 + 1024 * i, [1, 512], BF16) for i in range(2)]
    dma("pool", "biasA", biasA[:, :, :], biasA_in.rearrange("p (a b) -> p a b", b=512), (), ("biasA",))
    for i in range(2):
        dve(lambda e, i=i: e.memset(qas[i][64:65, 0, :], 1.0), (), ("qa1_%d" % i,))
        dve(lambda e, i=i: e.memset(vas[i][:, :, 64:65], 1.0), (), ("vao%d" % i,))

    def a_loads(h, p, sl):
        L = LA[p]; off = LA_OFF[p]
        qa, ka, va = qas[sl], kas[sl], vas[sl]
        qr, kr, vr = "qa%d" % sl, "ka%d" % sl, "va%d" % sl
        dma("sp", qr, qa[0:64, 0, 0:L], QT_A[h * 64:(h + 1) * 64, off:off + L], ("QT_A",), (qr,))
        dma("sp", kr, ka[0:64, 0, 0:L], KT_A[h * 64:(h + 1) * 64, off:off + L], ("KT_A",), (kr,))
        dma("pool", kr + "v", ka[64:65, 0, 0:L], kvalid[0:1, off:off + L], (), (kr + "v",))
        nb = L // 128
        for b0 in range(0, nb, 16):
            b1 = min(nb, b0 + 16)
            dma("sp", vr, va[:, b0:b1, 0:64],
                V_A[off + b0 * 128:off + b1 * 128, h * 64:(h + 1) * 64].rearrange("(b p) d -> p b d", p=128), ("V_A",), (vr,))

    units = []
    for h in range(8):
        for p in range(3):
            for b in range(16):
                units.append((h, p, b))
    a_loads(0, 0, 0)

    def a_qk(u):
        h, p, b = units[u]
        sl = (h * 3 + p) % 2
        L = LA[p]; dil = DIL[p]
        qa, ka = qas[sl], kas[sl]
        per = 16 // dil
        r = b // per; bb = b % per
        s0 = r * (L // dil) + 256 * bb
        sb = 4 + (u % 2)
        bi = 2 * p - h - 1 + 8

        def qk(e):
            e.matmul(ps[:, sb, :], lhsT=ident_b[:, :], rhs=biasA[:, bi, :], start=True, stop=False)
            e.matmul(ps[:, sb, 0:128], lhsT=ka[0:65, 0, s0:s0 + 128], rhs=qa[0:65, 0, s0 + 64:s0 + 192], start=False, stop=False)
            e.matmul(ps[:, sb, 128:384], lhsT=ka[0:65, 0, s0 + 128:s0 + 256], rhs=qa[0:65, 0, s0 + 64:s0 + 320], start=False, stop=False)
            return e.matmul(ps[:, sb, 384:512], lhsT=ka[0:65, 0, s0 + 256:s0 + 384], rhs=qa[0:65, 0, s0 + 192:s0 + 320], start=False, stop=True)
        pe(qk, ("qa%d" % sl, "qa1_%d" % sl, "ka%d" % sl, "ka%dv" % sl, "biasA", "ident_b"), ("ps%d" % sb,))

    def a_rest(u):
        h, p, b = units[u]
        sl = (h * 3 + p) % 2
        L = LA[p]; dil = DIL[p]
        va = vas[sl]
        per = 16 // dil
        r = b // per; bb = b % per
        s0 = r * (L // dil) + 256 * bb
        sb = 4 + (u % 2); ob = 6 + (u % 2); pt = pTa[u % 2]; ptr = "pTa%d" % (u % 2)
        kb0 = s0 // 128
        act(lambda e: e.activation(out=pt[:, 0, :], in_=ps[:, sb, :], func=AF.Exp), ("ps%d" % sb,), (ptr,))

        def pv(e):
            e.matmul(ps[0:65, ob, 0:256], lhsT=va[:, kb0 + 1, :], rhs=pt[:, 0, 128:384], start=True, stop=False)
            e.matmul(ps[0:65, ob, 0:128], lhsT=va[:, kb0, :], rhs=pt[:, 0, 0:128], start=False, stop=False)
            return e.matmul(ps[0:65, ob, 128:256], lhsT=va[:, kb0 + 2, :], rhs=pt[:, 0, 384:512], start=False, stop=True)
        return pv, ("va%d" % sl, "vao%d" % sl, ptr), ("ps%d" % ob,), ob, r + dil * 256 * bb, dil, p

    def a_norm(h):
        act(lambda e: e.activation(out=rl[64:65, 0, :], in_=accO[64:65, 0, :], func=AF.Ln), ("accO",), ("rl",))
        act(lambda e: e.activation(out=rl[64:65, 0, :], in_=rl[64:65, 0, :], func=AF.Exp, scale=-1.0), ("rl",), ("rl",))
        for j in range(8):
            bank = j % 4
            pe(lambda e, j=j, bank=bank: e.matmul(ps[:, bank, :], lhsT=ones_f[64:65, :], rhs=rl[64:65, 0, j * 512:(j + 1) * 512],
                                                  start=True, stop=True), ("rl", "ones_f"), ("ps%d" % bank,))
            ms = mst[j % 2]; msr = "mst%d" % (j % 2)
            dve(lambda e, j=j, bank=bank, ms=ms: e.tensor_tensor(out=ms[0:64, 0, :], in0=accO[0:64, 0, j * 512:(j + 1) * 512],
                                                                 in1=ps[0:64, bank, :], op=ALU.mult), ("accO", "ps%d" % bank), (msr,))
            dma("sp", msr, MIXT[h * 64:(h + 1) * 64, j * 512:(j + 1) * 512], ms[0:64, 0, :], (msr,), ("MIXT",))

    a_qk(0)
    for u in range(len(units)):
        h, p, b = units[u]
        if b == 0 and u + 16 < len(units):
            hn, pn, _ = units[u + 16]
            a_loads(hn, pn, (hn * 3 + pn) % 2)
        pvfn, pr, pw, ob, tstart, dil, p = a_rest(u)
        if u + 1 < len(units):
            a_qk(u + 1)
        pe(pvfn, pr, pw)
        dst = accO[0:65, 0, tstart:tstart + dil * 255 + 1:dil]
        if p == 0:
            dve(lambda e, dst=dst, ob=ob: e.tensor_copy(out=dst, in_=ps[0:65, ob, 0:256]), ("ps%d" % ob,), ("accO",))
        else:
            dve(lambda e, dst=dst, ob=ob: e.tensor_tensor(out=dst, in0=ps[0:65, ob, 0:256], in1=dst, op=ALU.add),
                ("ps%d" % ob, "accO"), ("accO",))
        if p == 2 and b == 15:
            a_norm(h)
    P.barrier()

    KTs = [view(32768 * c, [1, S], BF16) for c in range(2)]
    V1 = view(65536, [128, 129], BF16)
    QTv = [view(98560 + 6144 * i, [6, 512], BF16) for i in range(2)]
    pTb = [view(110848 + 2048 * i, [2, 512], BF16) for i in range(2)]
    accS = view(114944, [8, 129], F32)
    diagB = view(119072, [4, 128], BF16)
    odt = view(120096, [4, 128], F32)
    t1 = view(122144, [1, 128], F32)
    odn = view(122656, [4, 128], BF16)
    sq = view(123680, [1, 128], F32)
    mixst = [view(124192 + 1024 * i, [1, 512], BF16) for i in range(2)]
    stat = small("statB", [32])
    dma("pool", "diagB", diagB[:, :, :], diagB_in.rearrange("p (a b) -> p a b", b=128), (), ("diagB",))
    for c in range(2):
        dma("pool", "kaug%d" % c, KTs[c][64:68, 0, :], kaugB[:, :], (), ("KTa%d" % c,))
    dve(lambda e: e.memset(V1[:, :, 128:129], 1.0), (), ("V1o",))
    ACC_POS = [(0, 0), (0, 129), (0, 258), (1, 0), (1, 129), (1, 258), (2, 0), (2, 129)]

    def b_kvloads(h):
        for c in range(2):
            dma("sp", "KT%d" % c, KTs[c][0:64, 0, :], KT_B[h, c * 64:(c + 1) * 64, :], ("KT_B",), ("KT%d" % c,))
        for b0 in range(0, 128, 16):
            dma("sp", "V1", V1[:, b0:b0 + 16, 0:128],
                V_B[b0 * 128:(b0 + 16) * 128, h * 128:(h + 1) * 128].rearrange("(b p) d -> p b d", p=128), ("V_B",), ("V1",))

    def b_qloads(g):
        h, qb = g // 8, g % 8
        sl = g % 2
        qt = QTv[sl]; qtr = "QTv%d" % sl
        for var in range(3):
            for c in range(2):
                dma("sp", qtr, qt[0:64, var * 2 + c, :], QT_B[h, c * 64:(c + 1) * 64, qb * 512:(qb + 1) * 512], ("QT_B",), (qtr,))
        dma("pool", qtr + "a", qt[64:68, :, :], qaugB[h, qb].rearrange("r (v t) -> r v t", t=512), (), (qtr + "a",))

    def b_qk(g, kb, u):
        h, qb = g // 8, g % 8
        st_ = u % 2
        qt = QTv[g % 2]; qtr = "QTv%d" % (g % 2)

        def qk(e):
            i = None
            for c in range(2):
                o = ps[:, 2 * st_ + c, :]
                kt = KTs[c][0:68, 0, kb * 128:(kb + 1) * 128]
                if kb >= 32 or kb // 4 != qb:
                    var = 0 if (kb >= 32 or kb // 4 > qb) else 1
                    i = e.matmul(o, lhsT=kt, rhs=qt[0:68, var * 2 + c, :], start=True, stop=True)
                else:
                    j = kb % 4
                    if j > 0:
                        e.matmul(o[:, 0:128 * j], lhsT=kt, rhs=qt[0:68, 0 + c, 0:128 * j], start=True, stop=True)
                    if j < 3:
                        e.matmul(o[:, 128 * (j + 1):512], lhsT=kt, rhs=qt[0:68, 2 + c, 128 * (j + 1):512], start=True, stop=True)
                    e.matmul(o[:, 128 * j:128 * (j + 1)], lhsT=ident_b[:, :], rhs=diagB[:, h, :], start=True, stop=False)
                    i = e.matmul(o[:, 128 * j:128 * (j + 1)], lhsT=kt, rhs=qt[0:68, 4 + c, 128 * j:128 * (j + 1)],
                                 start=False, stop=True)
            return i
        pe(qk, ("KT0", "KT1", "KTa0", "KTa1", qtr, qtr + "a", "diagB", "ident_b"), ("psS%d" % st_,))

    def b_post(g):
        h, qb = g // 8, g % 8
        for a in range(8):
            bk, col = ACC_POS[a]
            if a % 2 == 0:
                dve(lambda e, a=a, bk=bk, col=col: e.tensor_copy(out=accS[:, a, :], in_=ps[:, 4 + bk, col:col + 129]), ("accP",), ("accS",))
            else:
                act(lambda e, a=a, bk=bk, col=col: e.activation(out=accS[:, a, :], in_=ps[:, 4 + bk, col:col + 129], func=AF.Copy), ("accP",), ("accS",))
        dve(lambda e: e.reciprocal(out=stat[:, 0:8], in_=accS[:, :, 128]), ("accS",), ("stat",))
        dve(lambda e: e.tensor_scalar(out=stat[:, 8:12], in0=stat[:, 4:8], scalar1=nlam[:, 0:1], scalar2=None, op0=ALU.mult), ("stat", "nlam"), ("stat",))
        for qs in range(4):
            dve(lambda e, qs=qs: e.tensor_scalar(out=t1[:, 0, :], in0=accS[:, qs, 0:128], scalar1=stat[:, qs:qs + 1], scalar2=None, op0=ALU.mult),
                ("accS", "stat"), ("t1",))
            dve(lambda e, qs=qs: e.scalar_tensor_tensor(out=odt[:, qs, :], in0=accS[:, 4 + qs, 0:128], scalar=stat[:, 8 + qs:9 + qs], in1=t1[:, 0, :],
                                                        op0=ALU.mult, op1=ALU.add), ("accS", "stat", "t1"), ("odt",))
            dve(lambda e, qs=qs: e.scalar_tensor_tensor(out=sq[:, 0, :], in0=odt[:, qs, :], scalar=1.0, in1=odt[:, qs, :], op0=ALU.mult, op1=ALU.mult,
                                                        accum_out=stat[:, 12 + qs:13 + qs]), ("odt",), ("sq", "stat"))
        act(lambda e: e.activation(out=stat[:, 16:20], in_=stat[:, 12:16], func=AF.Sqrt, bias=EPS, scale=1.0 / 128.0), ("stat",), ("stat",))
        dve(lambda e: e.reciprocal(out=stat[:, 20:24], in_=stat[:, 16:20]), ("stat",), ("stat",))
        ms = mixst[g % 2]; msr = "mixst%d" % (g % 2)
        for qs in range(4):
            dve(lambda e, qs=qs: e.scalar_tensor_tensor(out=odn[:, qs, :], in0=odt[:, qs, :], scalar=stat[:, 20 + qs:21 + qs], in1=gsc[:, :],
                                                        op0=ALU.mult, op1=ALU.mult), ("odt", "stat", "gsc"), ("odn",))
        tb = ps[:, 7, :].bitcast(BF16)

        def tp(e):
            i = None
            for qs in range(4):
                i = e.transpose(out=tb[:, qs * 128:(qs + 1) * 128], in_=odn[:, qs, :], identity=ident_b[:, :])
            return i
        pe(tp, ("odn", "ident_b"), ("ps7",))
        dve(lambda e, ms=ms: e.tensor_copy(out=ms[:, 0, :], in_=tb[:, 0:512]), ("ps7",), (msr,))
        dma("sp", msr, MIXT[512 + h * 128:512 + (h + 1) * 128, qb * 512:(qb + 1) * 512], ms[:, 0, :], (msr,), ("MIXT",))

    NG = 32

    def b_needed(h, qb, kb):
        dmax = 150.0 * (2.0 ** (2 * (h + 1)))
        q0, q1 = qb * 512, qb * 512 + 511
        k0, k1 = kb * 128, kb * 128 + 127
        if kb < 32:
            gap = max(0, k0 - q1, q0 - k1)
        else:
            gap = min(k0 - q1, q0 - (k1 - S))
        return gap <= dmax

    glist = [[kb for kb in range(128) if b_needed(g // 8, g % 8, kb)] for g in range(NG)]
    flat = [(g, i) for g in range(NG) for i in range(len(glist[g]))]
    b_kvloads(0)
    b_qloads(0)
    b_qk(0, glist[0][0], 0)
    for u, (g, i) in enumerate(flat):
        h, qb = g // 8, g % 8
        kb = glist[g][i]
        if i == 0 and g + 1 < NG:
            b_qloads(g + 1)
        st_ = u % 2
        pt = pTb[st_]; ptr = "pTb%d" % st_
        act(lambda e, pt=pt, st_=st_: e.activation(out=pt[:, :, :], in_=ps[:, 2 * st_:2 * st_ + 2, :], func=AF.Exp), ("psS%d" % st_,), (ptr,))
        if u + 1 < len(flat):
            gn, i_n = flat[u + 1]
            if gn != g and gn % 8 == 0:
                b_kvloads(gn // 8)
            b_qk(gn, glist[gn][i_n], u + 1)
        first = (i == 0); last = (i == len(glist[g]) - 1)

        def pv(e, kb=kb, pt=pt, first=first, last=last):
            ins = None
            for a in range(8):
                c, qs = a // 4, a % 4
                bk, col = ACC_POS[a]
                ins = e.matmul(ps[:, 4 + bk, col:col + 129], lhsT=pt[:, c, qs * 128:(qs + 1) * 128], rhs=V1[:, kb, :],
                               start=(first and col == 0), stop=last, skip_group_check=True)
            return ins
        pe(pv, (ptr, "V1", "V1o"), ("accP",))
        if last:
            b_post(g)
    P.barrier()

    woA = view(0, [8, 1024], BF16)
    woB = view(16384, [4, 1024], BF16)
    mixA = [view(24576 + 8192 * i, [8, 512], BF16) for i in range(2)]
    mixB = [view(40960 + 4096 * i, [4, 512], BF16) for i in range(2)]
    xo = [view(49152 + 4096 * i, [1, 1024], F32) for i in range(2)]
    h1 = [view(57344 + 4096 * i, [1, 1024], F32) for i in range(2)]
    x1t = [view(65536 + 4096 * i, [1, 1024], F32) for i in range(2)]
    x1b = [view(73728 + 2048 * i, [1, 1024], BF16) for i in range(2)]
    x1T = [view(77824 + 4096 * i, [8, 128], F32) for i in range(2)]
    lng = view(86016, [1, 1024], F32); lnb = view(90112, [1, 1024], F32)
    wr = view(94208, [8, 32], F32)
    sm = [[view(95232 + 1024 * i + 128 * k, [1, 32], F32) for k in range(6)] for i in range(2)]
    mskb = [view(97280 + 64 * i, [1, 32], BF16) for i in range(2)]
    top8 = [small("top8_%d" % i, [8]) for i in range(2)]
    bst = [small("bst%d" % i, [12]) for i in range(2)]
    mv = [small("mv%d" % i, [2]) for i in range(2)]
    st2 = [small("st2_%d" % i, [4]) for i in range(2)]
    dma("pool", "woA", woA[0:64, :, :], w_out[0:512, :].rearrange("(h d) f -> d h f", d=64), (), ("woA",))
    dma("pool", "woB", woB[:, :, :], w_out[512:1024, :].rearrange("(h d) f -> d h f", d=128), (), ("woB",))
    dma("sp", "lng", lng[:, 0, :], ln1_g.broadcast_to([128, D]), (), ("lng",))
    dma("sp", "lnb", lnb[:, 0, :], ln1_b.broadcast_to([128, D]), (), ("lnb",))
    dma("sp", "wr", wr[:, :, :], w_router.rearrange("(c p) e -> p c e", p=128), (), ("wr",))

    def layer_norm(src, dst, g_ap, b_ap, tag, res_src, res_dst, i2, on_act=False):
        b_, m_, s_ = bst[i2], mv[i2], st2[i2]
        bn, mn, sn = "bst%d" % i2, "mv%d" % i2, "st2_%d" % i2
        for i in range(2):
            dve(lambda e, i=i: e.bn_stats(out=b_[:, 6 * i:6 * (i + 1)], in_=src[:, 0, 512 * i:512 * (i + 1)]), (res_src,), (bn,))
        dve(lambda e: e.bn_aggr(out=m_[:, :], in_=b_[:, :]), (bn,), (mn,))
        act(lambda e: e.activation(out=s_[:, 0:1], in_=m_[:, 1:2], func=AF.Sqrt, bias=EPS, scale=1.0), (mn,), (sn,))
        dve(lambda e: e.reciprocal(out=s_[:, 1:2], in_=s_[:, 0:1]), (sn,), (sn,))
        dve(lambda e: e.tensor_scalar(out=dst[:, 0, :], in0=src[:, 0, :], scalar1=m_[:, 0:1], scalar2=s_[:, 1:2], op0=ALU.subtract, op1=ALU.mult),
            (res_src, mn, sn), (res_dst,))
        dve(lambda e: e.tensor_tensor(out=dst[:, 0, :], in0=dst[:, 0, :], in1=g_ap, op=ALU.mult), (res_dst, tag + "g"), (res_dst,))
        dve(lambda e: e.tensor_tensor(out=dst[:, 0, :], in0=dst[:, 0, :], in1=b_ap, op=ALU.add), (res_dst, tag + "b"), (res_dst,))

    def c_loads(tt):
        s2 = tt % 2
        if tt % 4 == 0:
            ms2 = (tt // 4) % 2
            dma("sp", "mixA%d" % ms2, mixA[ms2][0:64, :, :], MIXT[0:512, tt * 128:tt * 128 + 512].rearrange("(h d) t -> d h t", d=64), ("MIXT",), ("mixA%d" % ms2,))
            dma("sp", "mixB%d" % ms2, mixB[ms2][:, :, :], MIXT[512:1024, tt * 128:tt * 128 + 512].rearrange("(h d) t -> d h t", d=128), ("MIXT",), ("mixB%d" % ms2,))
        dma("sp", "xo%d" % s2, xo[s2][:, 0, :], x_own[tt * 128:(tt + 1) * 128, :], (), ("xo%d" % s2,))

    def c_stageA1(tt):
        s2 = tt % 2
        lg, msk, ex, gg, posf, junk = sm[s2]
        R = lambda n: "%s%d" % (n, s2)
        ms2 = (tt // 4) % 2
        tq = (tt % 4) * 128
        lg, msk, ex, gg, posf, junk = sm[s2]
        R = lambda n: "%s%d" % (n, s2)
        yb = 2 * s2

        def mmo(e, ms2=ms2, tq=tq, yb=yb):
            i = None
            for n in range(2):
                for hh in range(8):
                    i = e.matmul(ps[:, yb + n, :], lhsT=mixA[ms2][0:64, hh, tq:tq + 128], rhs=woA[0:64, hh, n * 512:(n + 1) * 512], start=(hh == 0), stop=False)
                for hh in range(4):
                    i = e.matmul(ps[:, yb + n, :], lhsT=mixB[ms2][:, hh, tq:tq + 128], rhs=woB[:, hh, n * 512:(n + 1) * 512], start=False, stop=(hh == 3))
            return i
        pe(mmo, ("mixA%d" % ms2, "mixB%d" % ms2, "woA", "woB"), (R("psY"),))
        dve(lambda e, s2=s2, yb=yb: e.scalar_tensor_tensor(out=h1[s2][:, 0, :], in0=xo[s2][:, 0, :], scalar=ALPHA, in1=ps[:, yb:yb + 2, :].rearrange("p a b -> p (a b)"),
                                                           op0=ALU.mult, op1=ALU.add), (R("xo"), R("psY")), (R("h1"),))

    def c_stageA2(tt):
        s2 = tt % 2
        R = lambda n: "%s%d" % (n, s2)
        layer_norm(h1[s2], x1t[s2], lng[:, 0, :], lnb[:, 0, :], "ln", R("h1"), R("x1t"), s2)
        dma("sp", R("x1w"), X1[tt * 128:(tt + 1) * 128, :], x1t[s2][:, 0, :], (R("x1t"),), ("X1",))
        act(lambda e, s2=s2: e.activation(out=x1b[s2][:, 0, :], in_=x1t[s2][:, 0, :], func=AF.Copy), (R("x1t"),), (R("x1b"),))

        def tpx(e, s2=s2):
            i = None
            for c in range(8):
                i = e.transpose(out=ps[:, 4 + c // 4, (c % 4) * 128:(c % 4 + 1) * 128], in_=x1t[s2][:, 0, c * 128:(c + 1) * 128], identity=ident_f[:, :])
            return i
        pe(tpx, (R("x1t"), "ident_f"), ("psT",))
        act(lambda e, s2=s2: e.activation(out=x1T[s2][:, :, :], in_=ps[:, 4:6, :].rearrange("p b (c s) -> p (b c) s", s=128), func=AF.Copy), ("psT",), (R("x1T"),))

    def c_stageB(tt):
        s2 = tt % 2
        lg, msk, ex, gg, posf, junk = sm[s2]
        R = lambda n: "%s%d" % (n, s2)
        lb = 6 + s2

        def mmr(e, s2=s2, lb=lb):
            i = None
            for c in range(8):
                i = e.matmul(ps[:, lb, 0:32], lhsT=x1T[s2][:, c, :], rhs=wr[:, c, :], start=(c == 0), stop=(c == 7))
            return i
        pe(mmr, (R("x1T"), "wr"), (R("psL"),))
        t8 = top8[s2]; s_ = st2[s2]; mb = mskb[s2]
        dve(lambda e, lg=lg, lb=lb: e.tensor_tensor(out=lg[:, 0, :], in0=ps[:, lb, 0:32], in1=brt[:, :], op=ALU.add), (R("psL"), "brt"), (R("lg"),))
        dve(lambda e, lg=lg, t8=t8: e.max(out=t8[:, :], in_=lg[:, 0, :]), (R("lg"),), (R("top8"),))
        dve(lambda e, lg=lg, t8=t8, msk=msk: e.tensor_scalar(out=msk[:, 0, :], in0=lg[:, 0, :], scalar1=t8[:, 3:4], scalar2=None, op0=ALU.is_ge), (R("lg"), R("top8")), (R("msk"),))
        dve(lambda e, msk=msk, mb=mb: e.tensor_copy(out=mb[:, 0, :], in_=msk[:, 0, :]), (R("msk"),), (R("mskb"),))
        dve(lambda e, t8=t8, s_=s_: e.tensor_scalar(out=s_[:, 2:3], in0=t8[:, 0:1], scalar1=-1.0, scalar2=None, op0=ALU.mult), (R("top8"),), (R("st2n"),))
        act(lambda e, lg=lg, ex=ex, s_=s_: e.activation(out=ex[:, 0, :], in_=lg[:, 0, :], func=AF.Exp, bias=s_[:, 2:3], scale=1.0), (R("lg"), R("st2n")), (R("ex"),))
        dve(lambda e, ex=ex, msk=msk, gg=gg, s_=s_: e.scalar_tensor_tensor(out=gg[:, 0, :], in0=ex[:, 0, :], scalar=1.0, in1=msk[:, 0, :], op0=ALU.mult, op1=ALU.mult,
                                                                         accum_out=s_[:, 3:4]), (R("ex"), R("msk")), (R("gg"), R("st2s")))
        dve(lambda e, s_=s_: e.reciprocal(out=s_[:, 3:4], in_=s_[:, 3:4]), (R("st2s"),), (R("st2s"),))
        dve(lambda e, gg=gg, s_=s_: e.tensor_scalar(out=gg[:, 0, :], in0=gg[:, 0, :], scalar1=s_[:, 3:4], scalar2=None, op0=ALU.mult), (R("gg"), R("st2s")), (R("gg"),))

        def mmk(e, mb=mb, lb=lb):
            e.matmul(ps[:, lb, 32:64], lhsT=ltri_b[:, :], rhs=mb[:, 0, :], start=True, stop=False)
            return e.matmul(ps[:, lb, 32:64], lhsT=ones_b[:, :], rhs=cum_b[:, :], start=False, stop=True)
        pe(mmk, (R("mskb"), "cum_b", "ltri_b", "ones_b", R("lg")), (R("psK"),))
        dve(lambda e, posf=posf, lb=lb: e.tensor_scalar(out=posf[:, 0, :], in0=ps[:, lb, 32:64], scalar1=float(CAP - 1), scalar2=None, op0=ALU.min), (R("psK"),), (R("posf"),))
        dve(lambda e, posf=posf: e.tensor_tensor(out=posf[:, 0, :], in0=posf[:, 0, :], in1=eoff[:, :], op=ALU.add), (R("posf"), "eoff"), (R("posf"),))
        dve(lambda e, msk=msk: e.tensor_tensor(out=cum_f[:, :], in0=cum_f[:, :], in1=msk[:, 0, :], op=ALU.add), ("cum_f", R("msk")), ("cum_f",))
        dve(lambda e: e.tensor_copy(out=cum_b[:, :], in_=cum_f[:, :]), ("cum_f",), ("cum_b",))
        for k in range(4):
            dve(lambda e, k=k, tt=tt, lg=lg, t8=t8, posf=posf, junk=junk: e.scalar_tensor_tensor(
                out=junk[:, 0, :], in0=lg[:, 0, :], scalar=t8[:, k:k + 1], in1=posf[:, 0, :],
                op0=ALU.is_equal, op1=ALU.mult, accum_out=sl_f[:, tt * 4 + k:tt * 4 + k + 1]),
                (R("lg"), R("top8"), R("posf")), (R("junk"), "sl_f%d" % tt))
            dve(lambda e, k=k, tt=tt, lg=lg, t8=t8, gg=gg, junk=junk: e.scalar_tensor_tensor(
                out=junk[:, 0, :], in0=lg[:, 0, :], scalar=t8[:, k:k + 1], in1=gg[:, 0, :],
                op0=ALU.is_equal, op1=ALU.mult, accum_out=gatek[:, tt * 4 + k:tt * 4 + k + 1]),
                (R("lg"), R("top8"), R("gg")), (R("junk"), "gatek%d" % tt))
        dve(lambda e, tt=tt: e.tensor_copy(out=sl_i[:, tt * 4:tt * 4 + 4], in_=sl_f[:, tt * 4:tt * 4 + 4]), ("sl_f%d" % tt,), ("sl_i%d" % tt,))
        for k in range(4):
            P.add("pool", lambda e, k=k, tt=tt, s2=s2: e.indirect_dma_start(
                out=XE[:, :], out_offset=bass.IndirectOffsetOnAxis(ap=sl_i[:, tt * 4 + k:tt * 4 + k + 1], axis=0),
                in_=x1b[s2][:, 0, :], in_offset=None), (R("x1b"), "sl_i%d" % tt), ("XE",), chan="sc%d" % s2)

    c_loads(0)
    c_loads(1)
    c_stageA1(0)
    c_loads(2)
    c_stageA1(1)
    c_stageA2(0)
    for tt in range(NT):
        if tt + 3 < NT:
            c_loads(tt + 3)
        if tt + 2 < NT:
            c_stageA1(tt + 2)
        if tt + 1 < NT:
            c_stageA2(tt + 1)
        c_stageB(tt)
    P.barrier()

    wu = [view(32768 * i, [8, 2048], BF16) for i in range(2)]
    wd = [view(65536 + 16384 * i, [8, 1024], BF16) for i in range(2)]
    NST = CAP // 128
    xe = [view(98304, [NST, 1024], BF16)] * 2
    xeT = [view(110592 + 12288 * i, [8, CAP], BF16) for i in range(2)]
    aT = [view(135168 + 12288 * i, [8, CAP], BF16) for i in range(2)]
    tmp = [view(159744 + 1536 * i, [1, CAP // 2], F32) for i in range(8)]
    ysb = [view(172032 + 4096 * i, [1, 1024], F32) for i in range(2)]
    bdn = [view(180224 + 4096 * i, [1, 1024], F32) for i in range(2)]
    bup = view(188416, [NE, 16], F32)
    dma("sp", "bup", bup[:, :, :], b_upT.rearrange("p (e c) -> p e c", c=16), (), ("bup",))
    HALF = CAP // 2
    ectr = {"gi": 0, "yi": 0}

    def e_load(ex_):
        s2 = ex_ % 2
        for c in range(8):
            dma("pool", "wu%d" % s2, wu[s2][:, c, :], w_up[ex_, c * 128:(c + 1) * 128, :], (), ("wu%d" % s2,))
        for c in range(8):
            dma("pool", "wd%d" % s2, wd[s2][:, c, :], w_down[ex_, c * 128:(c + 1) * 128, :], (), ("wd%d" % s2,))
        dma("sp", "bdn%d" % s2, bdn[s2][:, 0, :], b_down[ex_:ex_ + 1, :].broadcast_to([128, D]), (), ("bdn%d" % s2,))

    def e_load_x(ex_):
        s2 = ex_ % 2
        dma("sp", "xe", xe[s2][:, :, :], XE[ex_ * CAP:(ex_ + 1) * CAP, :].rearrange("(s p) d -> p s d", p=128), ("XE",), ("xe",))

    def e_T(ex_):
        s2 = ex_ % 2
        for st_ in range(NST):
            for cg in range(2):
                bank = (st_ * 2 + cg) % 2
                tb = ps[:, bank, :].bitcast(BF16)

                def tpe(e, st_=st_, cg=cg, tb=tb):
                    i = None
                    for cc in range(4):
                        c = cg * 4 + cc
                        i = e.transpose(out=tb[:, cc * 128:(cc + 1) * 128], in_=xe[s2][:, st_, c * 128:(c + 1) * 128], identity=ident_b[:, :])
                    return i
                pe(tpe, ("xe", "ident_b"), ("psE%d" % bank,))
                if (st_ + cg) % 2 == 0:
                    dve(lambda e, st_=st_, cg=cg, tb=tb: e.tensor_copy(
                        out=xeT[s2][:, cg * 4:cg * 4 + 4, st_ * 128:(st_ + 1) * 128], in_=tb[:, 0:512].rearrange("p (c s) -> p c s", s=128)),
                        ("psE%d" % bank,), ("xeT%d" % s2,))
                else:
                    act(lambda e, st_=st_, cg=cg, tb=tb: e.activation(
                        out=xeT[s2][:, cg * 4:cg * 4 + 4, st_ * 128:(st_ + 1) * 128], in_=tb[:, 0:512].rearrange("p (c s) -> p c s", s=128), func=AF.Copy),
                        ("psE%d" % bank,), ("xeT%d" % s2,))

    def e_U(ex_):
        s2 = ex_ % 2
        for j in range(8):
            for hf in range(2):
                gs = ectr["gi"] % 2; ectr["gi"] += 1
                bg, bu = 2 + 2 * gs, 3 + 2 * gs
                c0 = hf * HALF

                def up(e, j=j, c0=c0, bg=bg, bu=bu):
                    i = None
                    for (bank, fc) in ((bg, j), (bu, j + 8)):
                        for c in range(8):
                            i = e.matmul(ps[:, bank, 0:HALF], lhsT=wu[s2][:, c, fc * 128:(fc + 1) * 128], rhs=xeT[s2][:, c, c0:c0 + HALF],
                                         start=(c == 0), stop=(c == 7))
                    return i
                pe(up, ("wu%d" % s2, "xeT%d" % s2), ("psU%d" % gs,))
                gl, sg, ul, a1 = tmp[4 * gs], tmp[4 * gs + 1], tmp[4 * gs + 2], tmp[4 * gs + 3]
                tg = "tmp%d" % gs
                dve(lambda e, gl=gl, bg=bg, j=j: e.tensor_scalar(out=gl[:, 0, :], in0=ps[:, bg, 0:HALF], scalar1=bup[:, ex_, j:j + 1], scalar2=7.0,
                                                                 op0=ALU.add, op1=ALU.min), ("psU%d" % gs, "bup"), (tg + "gl",))
                act(lambda e, gl=gl, sg=sg: e.activation(out=sg[:, 0, :], in_=gl[:, 0, :], func=AF.Sigmoid, scale=1.702), (tg + "gl",), (tg + "sg",))
                dve(lambda e, ul=ul, bu=bu, j=j: e.tensor_scalar(out=ul[:, 0, :], in0=ps[:, bu, 0:HALF], scalar1=bup[:, ex_, 8 + j:9 + j], scalar2=7.0,
                                                                 op0=ALU.add, op1=ALU.min), ("psU%d" % gs, "bup"), (tg + "ul",))
                dve(lambda e, ul=ul: e.tensor_scalar(out=ul[:, 0, :], in0=ul[:, 0, :], scalar1=-7.0, scalar2=1.0, op0=ALU.max, op1=ALU.add),
                    (tg + "ul",), (tg + "ul",))
                dve(lambda e, gl=gl, sg=sg, a1=a1: e.tensor_tensor(out=a1[:, 0, :], in0=gl[:, 0, :], in1=sg[:, 0, :], op=ALU.mult),
                    (tg + "gl", tg + "sg"), (tg + "a1",))
                dve(lambda e, a1=a1, ul=ul, j=j, c0=c0: e.tensor_tensor(out=aT[s2][:, j, c0:c0 + HALF], in0=a1[:, 0, :], in1=ul[:, 0, :], op=ALU.mult),
                    (tg + "a1", tg + "ul"), ("aT%d" % s2,))

    def e_D(ex_):
        s2 = ex_ % 2
        for st_ in range(NST):
            ys = ysb[ectr["yi"] % 2]; ysr = "ysb%d" % (ectr["yi"] % 2); ectr["yi"] += 1
            for n in range(2):
                bank = 6 + n

                def dn(e, st_=st_, n=n, bank=bank):
                    i = None
                    for j in range(8):
                        i = e.matmul(ps[:, bank, :], lhsT=aT[s2][:, j, st_ * 128:(st_ + 1) * 128], rhs=wd[s2][:, j, n * 512:(n + 1) * 512],
                                     start=(j == 0), stop=(j == 7))
                    return i
                pe(dn, ("aT%d" % s2, "wd%d" % s2), ("psD%d" % n,))
                dve(lambda e, ys=ys, n=n, bank=bank: e.tensor_tensor(out=ys[:, 0, n * 512:(n + 1) * 512], in0=ps[:, bank, :],
                                                                    in1=bdn[s2][:, 0, n * 512:(n + 1) * 512], op=ALU.add),
                    ("psD%d" % n, "bdn%d" % s2), (ysr,))
            dma("sp", ysr, YS[ex_ * CAP + st_ * 128:ex_ * CAP + (st_ + 1) * 128, :], ys[:, 0, :], (ysr,), ("YS",))

    e_load(0)
    e_load(1)
    e_load_x(0)
    e_T(0)
    e_load_x(1)
    e_U(0)
    for ex_ in range(NE):
        if ex_ + 1 < NE:
            e_T(ex_ + 1)
        if ex_ + 2 < NE:
            e_load_x(ex_ + 2)
        e_D(ex_)
        if ex_ + 2 < NE:
            e_load(ex_ + 2)
        if ex_ + 1 < NE:
            e_U(ex_ + 1)
    P.barrier()

    yk = [view(16384 * i, [4, 1024], F32) for i in range(2)]
    x1r = [view(32768 + 4096 * i, [1, 1024], F32) for i in range(2)]
    acc = [view(40960 + 4096 * i, [1, 1024], F32) for i in range(2)]
    outt = [view(49152 + 4096 * i, [1, 1024], F32) for i in range(2)]
    l2g = view(57344, [1, 1024], F32); l2b = view(61440, [1, 1024], F32)
    dma("sp", "l2g", l2g[:, 0, :], ln2_g.broadcast_to([128, D]), (), ("l2g",))
    dma("sp", "l2b", l2b[:, 0, :], ln2_b.broadcast_to([128, D]), (), ("l2b",))
    out_ops = []

    def f_loads(tt):
        s2 = tt % 2
        for k in range(4):
            P.add("pool", lambda e, k=k, tt=tt, s2=s2: e.indirect_dma_start(
                out=yk[s2][:, k, :], out_offset=None, in_=YS[:, :],
                in_offset=bass.IndirectOffsetOnAxis(ap=sl_i[:, tt * 4 + k:tt * 4 + k + 1], axis=0)),
                ("YS", "sl_i%d" % tt), ("yk%d_%d" % (s2, k),), chan="yk%d_%d" % (s2, k))
        dma("sp", "x1r%d" % s2, x1r[s2][:, 0, :], X1[tt * 128:(tt + 1) * 128, :], ("X1",), ("x1r%d" % s2,))

    f_loads(0)
    for tt in range(NT):
        s2 = tt % 2
        if tt + 1 < NT:
            f_loads(tt + 1)
        a = acc[s2]; ar = "acc%d" % s2
        act(lambda e, a=a, s2=s2: e.activation(out=a[:, 0, :], in_=x1r[s2][:, 0, :], func=AF.Copy, scale=ALPHA), ("x1r%d" % s2,), (ar,))
        for k in range(4):
            dve(lambda e, a=a, s2=s2, tt=tt, k=k: e.scalar_tensor_tensor(out=a[:, 0, :], in0=yk[s2][:, k, :], scalar=gatek[:, tt * 4 + k:tt * 4 + k + 1],
                                                                        in1=a[:, 0, :], op0=ALU.mult, op1=ALU.add),
                ("yk%d_%d" % (s2, k), "gatek%d" % tt, ar), (ar,))
        layer_norm(a, outt[s2], l2g[:, 0, :], l2b[:, 0, :], "l2", ar, "outt%d" % s2, s2)
        out_ops.append(dma("sp", "out%d" % s2, out_d[tt * 128:(tt + 1) * 128, :], outt[s2][:, 0, :], ("outt%d" % s2,), ("out",)))
    fin = P.add("sp", None, ("out",), ())
    fin.raw = set(out_ops)

    P.prepare(nc)
    with nc.Block() as block:
        @block.tensor
        def _(e):
            P.emit_engine("pe", e)

        @block.scalar
        def _(e):
            P.emit_engine("act", e)

        @block.vector
        def _(e):
            P.emit_engine("dve", e)

        @block.gpsimd
        def _(e):
            P.emit_engine("pool", e)

        @block.sync
        def _(e):
            P.emit_engine("sp", e)
    return nc


def _tables(core):
    qr = core % 4
    base = qr * OWN
    pos = []
    pos.append(base - 64 + np.arange(LA[0]))
    for r in range(4):
        pos.append(base + r + 4 * (np.arange(1152) - 64))
    for r in range(16):
        pos.append(base + r + 16 * (np.arange(384) - 64))
    posA = np.concatenate(pos)
    validA = (posA >= 0) & (posA < S)
    kvalid = np.where(validA, 0.0, NEG).astype(np.float32)[None, :]
    posA = np.mod(posA, S)
    kpos = np.mod(base + np.arange(S), S)
    sig = np.ones(S, np.float32)
    other = np.arange(S) >= OWN
    sig[other & (kpos < base)] = -1.0
    kaug = np.stack([sig, sig, sig * (kpos // 128), sig * (kpos % 128)]).astype(np.float32)
    qpos = base + np.arange(OWN)
    qaug = np.zeros((4, 8, 4, 3, 2, 512), np.float32)
    for h in range(4):
        sl = 2.0 ** (-8.0 * (h + 1) / 4)
        for vi, sg in enumerate((1.0, -1.0, 0.0)):
            rows = np.stack([sg * sl * 128.0 * (qpos // 128), sg * sl * (qpos % 128),
                             np.full(OWN, -sg * sl * 128.0), np.full(OWN, -sg * sl)])
            rows = rows.reshape(4, 8, 512).transpose(1, 0, 2)
            qaug[h, :, :, vi, 0, :] = rows
            qaug[h, :, :, vi, 1, :] = rows
    return posA, kvalid, kpos, kaug, qaug.reshape(4, 8, 4, 3 * 2 * 512)


def _const_tables():
    ident = np.eye(128, dtype=np.float32)
    ltri = (np.arange(128)[:, None] < np.arange(128)[None, :]).astype(np.float32)
    ones = np.ones((128, 128), np.float32)
    eoff = np.zeros((128, 128), np.float32)
    eoff[:, 0:32] = (np.arange(32) * CAP)[None, :]
    zer = np.zeros((128, 128), np.float32)
    consts = np.concatenate([ident, ltri, ones, eoff, zer], axis=1)
    i = np.arange(128)
    diag = np.zeros((128, 4, 128), np.float32)
    for h in range(4):
        sl = 2.0 ** (-8.0 * (h + 1) / 4)
        diag[:, h, :] = -sl * np.abs(i[:, None] - i[None, :])
    bias = np.full((128, 12, 512), NEG, np.float32)
    kk = np.arange(128)[:, None]
    for idx in range(12):
        sd = 2.0 ** (idx - 8)
        for (c0, c1, joff, ioff) in ((0, 128, 0, 0), (128, 384, 128, 0), (384, 512, 256, 128)):
            cols = np.arange(c0, c1)[None, :]
            ii = cols - c0 + ioff
            dd = (kk + joff) - 64 - ii
            ok = np.abs(dd) <= 64
            bias[:, idx, c0:c1] = np.where(ok, -sd * np.abs(dd), NEG)
    return consts, diag.reshape(128, 512), bias.reshape(128, 12 * 512)


def _prep_inputs(inputs, cores):
    f = lambda a: np.ascontiguousarray(np.asarray(a, dtype=np.float32))
    x = f(inputs["x"])
    consts, diag, biasA = _const_tables()
    shared = {
        "w_in": f(inputs["w_in"][0]), "w_out": f(inputs["w_out"][0]),
        "w_router": f(inputs["w_router"][0]), "b_router": f(inputs["b_router"]),
        "w_up": f(inputs["w_up"][0]), "w_down": f(inputs["w_down"][0]),
        "b_upT": f(np.asarray(inputs["b_up"][0]).reshape(NE, 16, 128).transpose(2, 0, 1).reshape(128, NE * 16)),
        "b_down": f(inputs["b_down"][0]),
        "ln1_g": f(inputs["ln1_g"]), "ln1_b": f(inputs["ln1_b"]), "ln2_g": f(inputs["ln2_g"]), "ln2_b": f(inputs["ln2_b"]),
        "lam_in": f(np.concatenate([np.asarray(inputs[k][0]) for k in ("lambda_q1", "lambda_k1", "lambda_q2", "lambda_k2")])[None, :]),
        "gsub": f(inputs["diff_norm_g"]),
        "diagB": diag, "biasA": biasA, "consts": consts,
    }
    maps = []
    for core in cores:
        b = core // 4
        base = (core % 4) * OWN
        posA, kvalid, kpos, kaug, qaug = _tables(core)
        xb = x[b]
        m = dict(shared)
        m["xT_all"] = np.ascontiguousarray(xb[kpos].T)
        m["xTa"] = np.ascontiguousarray(xb[posA].T)
        m["x_own"] = np.ascontiguousarray(xb[base:base + OWN])
        m["kvalid"] = kvalid
        m["kaugB"] = kaug
        m["qaugB"] = qaug
        maps.append(m)
    return maps


_NC_CACHE = {}


def kernel(**inputs):
    if "nc" not in _NC_CACHE:
        _NC_CACHE["nc"] = build_program()
    nc = _NC_CACHE["nc"]
    cores = list(range(NCORES))
    maps = _prep_inputs(inputs, cores)
    res = run_bass_kernel_spmd(nc, maps, core_ids=cores)
    out = np.zeros((2, S, D), np.float32)
    for core in cores:
        b = core // 4
        base = (core % 4) * OWN
        out[b, base:base + OWN] = res.results[core]["out"]
    return out
```

```python
import math
import numpy as np
import concourse.bass as bass
import concourse.mybir as mybir
from concourse.bass_utils import run_bass_kernel_spmd
from concourse.alu_op_type import AluOpType as ALU

F32, BF16, I32 = mybir.dt.float32, mybir.dt.bfloat16, mybir.dt.int32
AF = mybir.ActivationFunctionType

NCORES = 8
S = 16384
D = 1024
OWN = 4096
NT = OWN // 128
CAP = 768
NE = 32
ALPHA = 2.0 ** 0.25
EPS = 1e-5
LAM_INIT = 0.8 - 0.6 * math.exp(0.0)
NEG = -30000.0
LA = (4224, 4608, 6144)
LA_OFF = (0, 4224, 8832)
LAT = sum(LA)
DIL = (1, 4, 16)

ENGS = ("pe", "act", "dve", "pool", "sp")


class Op:
    __slots__ = ("eng", "emit", "raw", "war", "signal", "val", "semkey", "dma", "waits", "idx")


class Prog:
    def __init__(self):
        self.ops = []
        self.lastw = {}
        self.readers = {}
        self.phase = 0
        self.fence = None
        self.last_by_key = {}

    def add(self, eng, emit, reads=(), writes=(), chan=None):
        op = Op()
        op.eng, op.emit, op.dma = eng, emit, chan
        op.semkey = ("dma", chan) if chan is not None else (eng, self.phase)
        op.signal = chan is not None
        op.val = 0
        op.idx = len(self.ops)
        raw, war = set(), set()
        for r in reads:
            raw.update(self.lastw.get(r, {}).values())
        for r in writes:
            if not r[0].isupper():
                raw.update(self.lastw.get(r, {}).values())
            war.update(self.readers.get(r, {}).values())
        if self.fence is not None:
            raw.add(self.fence)
        for r in reads:
            self.readers.setdefault(r, {})[op.semkey] = op
        for r in writes:
            if r[0].isupper():
                self.lastw.setdefault(r, {})[op.semkey] = op
            else:
                self.lastw[r] = {op.semkey: op}
            self.readers[r] = {}
        op.raw, op.war = raw, war
        self.ops.append(op)
        self.last_by_key[op.semkey] = op
        return op

    def barrier(self):
        deps = list(self.last_by_key.values())
        op = self.add("dve", None)
        op.raw = set(d for d in deps if d is not op)
        if self.fence is not None:
            op.raw.add(self.fence)
        self.fence = op
        self.phase += 1
        return op

    def finalize(self):
        for op in self.ops:
            best = {}
            for x, is_raw in [(x, True) for x in op.raw] + [(x, False) for x in op.war]:
                if x is op:
                    continue
                if x.dma is None and x.eng == op.eng and op.dma is None:
                    if op.eng == "pe":
                        continue
                    if not is_raw:
                        continue
                if x.dma is not None and op.dma is not None and x.semkey == op.semkey:
                    continue
                k = x.semkey
                if k not in best or best[k].idx < x.idx:
                    best[k] = x
            op.waits = list(best.values())
            for x in op.waits:
                x.signal = True
        counters = {}
        for op in self.ops:
            if op.signal:
                inc = 16 if op.dma is not None else 1
                counters[op.semkey] = counters.get(op.semkey, 0) + inc
                op.val = counters[op.semkey]
        return counters

    def prepare(self, nc):
        counters = self.finalize()
        self.sems = {k: nc.alloc_semaphore("s_%s_%s" % (str(k[0]), str(k[1]))) for k in counters}

    def emit_engine(self, e, eng):
        sems = self.sems
        wd = {}
        for op in self.ops:
            if op.eng != e:
                continue
            for x in op.waits:
                if wd.get(x.semkey, 0) >= x.val:
                    continue
                eng.wait_ge(sems[x.semkey], x.val)
                wd[x.semkey] = x.val
            ins = None
            if op.emit is not None:
                ins = op.emit(eng)
            if op.signal:
                if ins is None:
                    ins = eng.nop()
                ins.then_inc(sems[op.semkey], 16 if op.dma is not None else 1)


def build_program(debug=False):
    nc = bass.Bass("TRN2", target_bir_lowering=False)
    P = Prog()

    def din(name, shape, dt=F32):
        return nc.dram_tensor(name, list(shape), dt, kind="ExternalInput").ap()

    def dscr(name, shape, dt):
        return nc.dram_tensor(name, list(shape), dt, kind=("ExternalOutput" if debug else "Internal")).ap()

    xT_all = din("xT_all", [D, S])
    xTa = din("xTa", [D, LAT])
    x_own = din("x_own", [OWN, D])
    w_in = din("w_in", [D, 3072])
    w_out = din("w_out", [D, D])
    w_router = din("w_router", [D, NE])
    b_router = din("b_router", [1, NE])
    w_up = din("w_up", [NE, D, 2048])
    b_upT = din("b_upT", [128, NE * 16])
    w_down = din("w_down", [NE, D, D])
    b_down = din("b_down", [NE, D])
    ln1_g = din("ln1_g", [1, D]); ln1_b = din("ln1_b", [1, D])
    ln2_g = din("ln2_g", [1, D]); ln2_b = din("ln2_b", [1, D])
    lam_in = din("lam_in", [1, 256])
    gsub = din("gsub", [1, 128])
    kvalid = din("kvalid", [1, LAT])
    kaugB = din("kaugB", [4, S])
    qaugB = din("qaugB", [4, 8, 4, 3 * 2 * 512])
    diagB_in = din("diagB", [128, 4 * 128])
    biasA_in = din("biasA", [128, 12 * 512])
    consts = din("consts", [128, 5 * 128])
    out_d = nc.dram_tensor("out", [OWN, D], F32, kind="ExternalOutput").ap()

    KT_B = dscr("KT_B", [4, 128, S], BF16)
    V_B = dscr("V_B", [S, 512], BF16)
    QT_B = dscr("QT_B", [4, 128, OWN], BF16)
    QT_A = dscr("QT_A", [512, LAT], BF16)
    KT_A = dscr("KT_A", [512, LAT], BF16)
    V_A = dscr("V_A", [LAT, 512], BF16)
    MIXT = dscr("MIXT", [D, OWN], BF16)
    X1 = dscr("X1", [OWN, D], F32)
    XE = dscr("XE", [NE * CAP, D], BF16)
    YS = dscr("YS", [NE * CAP, D], F32)

    ARENA_BYTES = 188 * 1024
    arena = nc.alloc_sbuf_tensor("arena", [128, ARENA_BYTES // 2], BF16)
    ps = nc.alloc_psum_tensor("ps", [128, 8, 512], F32)

    def view(off, shape, dt, p0=0, p1=128):
        nbytes = int(np.prod(shape)) * (4 if dt in (F32, I32) else 2)
        assert off % 32 == 0 and off + nbytes <= ARENA_BYTES, (off, nbytes)
        a = arena[p0:p1, off // 2: (off + nbytes) // 2]
        if dt != BF16:
            a = a.bitcast(dt)
        if len(shape) == 2:
            return a.rearrange("p (a b) -> p a b", b=shape[1])
        if len(shape) == 3:
            return a.rearrange("p (a b c) -> p a b c", b=shape[1], c=shape[2])
        return a

    def small(name, shape, dt=F32):
        return nc.alloc_sbuf_tensor(name, [128] + list(shape), dt)

    ident_f = small("ident_f", [128]); ltri_f = small("ltri_f", [128]); ones_f = small("ones_f", [128])
    ident_b = small("ident_b", [128], BF16); ltri_b = small("ltri_b", [128], BF16); ones_b = small("ones_b", [128], BF16)
    eoff = small("eoff", [32])
    lamv = small("lamv", [256]); lamp = small("lamp", [128]); lam2 = small("lam2", [4])
    nlam = small("nlam", [1])
    gsc = small("gsc", [128])
    brt = small("brt", [32])
    sl_f = small("sl_f", [NT * 4]); sl_i = small("sl_i", [NT * 4], I32); gatek = small("gatek", [NT * 4])
    cum_f = small("cum_f", [32]); cum_b = small("cum_b", [32], BF16)
    scr1 = small("scr1", [8])

    def dma(eng, chan, out, in_, reads=(), writes=()):
        if eng == "pool":
            return P.add(eng, lambda e: e.dma_start(out=out, in_=in_, max_dma_last_dim=8192), reads, writes, chan=chan)
        return P.add(eng, lambda e: e.dma_start(out=out, in_=in_), reads, writes, chan=chan)

    def pe(fn, reads=(), writes=()):
        return P.add("pe", fn, reads, writes)

    def act(fn, reads=(), writes=()):
        return P.add("act", fn, reads, writes)

    def dve(fn, reads=(), writes=()):
        return P.add("dve", fn, reads, writes)

    dma("sp", "c0", ident_f[:, :], consts[:, 0:128], (), ("ident_f",))
    dma("sp", "c1", ltri_f[:, :], consts[:, 128:256], (), ("ltri_f",))
    dma("sp", "c2", ones_f[:, :], consts[:, 256:384], (), ("ones_f",))
    dma("sp", "c3", eoff[:, :], consts[:, 384:416], (), ("eoff",))
    dma("sp", "c4", lamv[:, :], lam_in.broadcast_to([128, 256]), (), ("lamv",))
    dma("sp", "c5", gsc[:, :], gsub.broadcast_to([128, 128]), (), ("gsc",))
    dma("sp", "c6", brt[:, :], b_router.broadcast_to([128, NE]), (), ("brt",))
    dve(lambda e: e.tensor_copy(out=ident_b[:, :], in_=ident_f[:, :]), ("ident_f",), ("ident_b",))
    dve(lambda e: e.tensor_copy(out=ltri_b[:, :], in_=ltri_f[:, :]), ("ltri_f",), ("ltri_b",))
    dve(lambda e: e.tensor_copy(out=ones_b[:, :], in_=ones_f[:, :]), ("ones_f",), ("ones_b",))
    dve(lambda e: e.memset(cum_f[:, :], 0.0), (), ("cum_f",))
    dve(lambda e: e.memset(cum_b[:, :], 0.0), (), ("cum_b",))
    dve(lambda e: e.tensor_tensor(out=lamp[:, 0:64], in0=lamv[:, 0:64], in1=lamv[:, 64:128], op=ALU.mult), ("lamv",), ("lamp",))
    dve(lambda e: e.tensor_tensor(out=lamp[:, 64:128], in0=lamv[:, 128:192], in1=lamv[:, 192:256], op=ALU.mult), ("lamv", "lamp"), ("lamp",))
    dve(lambda e: e.reduce_sum(out=lam2[:, 0:1], in_=lamp[:, 0:64], axis=mybir.AxisListType.X), ("lamp",), ("lam2a",))
    dve(lambda e: e.reduce_sum(out=lam2[:, 1:2], in_=lamp[:, 64:128], axis=mybir.AxisListType.X), ("lamp",), ("lam2b",))
    act(lambda e: e.activation(out=lam2[:, 2:4], in_=lam2[:, 0:2], func=AF.Exp), ("lam2a", "lam2b"), ("lam2c",))
    dve(lambda e: e.tensor_tensor(out=nlam[:, :], in0=lam2[:, 3:4], in1=lam2[:, 2:3], op=ALU.subtract), ("lam2c",), ("nlam",))
    dve(lambda e: e.tensor_scalar(out=nlam[:, :], in0=nlam[:, :], scalar1=-LAM_INIT, scalar2=None, op0=ALU.add), ("nlam",), ("nlam",))
    dve(lambda e: e.tensor_scalar(out=gsc[:, :], in0=gsc[:, :], scalar1=1.0 - LAM_INIT, scalar2=None, op0=ALU.mult), ("gsc",), ("gsc",))
    P.barrier()

    win = view(0, [8, 3072], BF16)
    SUP = 2048
    xts = [view(49152 + 32768 * i, [8, SUP], BF16) for i in range(2)]
    NSTG = 6
    stgs = [view(114688 + 1024 * i, [1, 512], BF16) for i in range(NSTG)]
    for c in range(8):
        dma("pool", "win", win[:, c, :], w_in[c * 128:(c + 1) * 128, :], (), ("win",))
    cnt = {"chunk": 0, "grp": 0}

    def proj_stream(src, L, outs):
        for u0 in range(0, L, SUP):
            TS = min(SUP, L - u0)
            ci = cnt["chunk"]; cnt["chunk"] += 1
            xt = xts[ci % 2]
            xr = "xt%d" % (ci % 2)
            for c in range(8):
                dma("pool", xr, xt[:, c, 0:TS], src[c * 128:(c + 1) * 128, u0:u0 + TS], (), (xr,))
            for s0 in range(0, TS, 512):
                t0 = u0 + s0
                T = min(512, TS - s0)
                for o in outs:
                    if t0 >= o["tmax"]:
                        continue
                    if o["kind"] == "fm":
                        for ft in range(o["nf"] // 128):
                            g = cnt["grp"]; cnt["grp"] += 1
                            bank = g % 8
                            pb = "ps%d" % bank
                            st = stgs[g % NSTG]; sr = "stg%d" % (g % NSTG)
                            f0 = o["w0"] + ft * 128

                            def mm(e, xt=xt, bank=bank, f0=f0, T=T, s0=s0):
                                for c in range(8):
                                    i = e.matmul(ps[:, bank, 0:T], lhsT=win[:, c, f0:f0 + 128], rhs=xt[:, c, s0:s0 + T],
                                                 start=(c == 0), stop=(c == 7))
                                return i
                            pe(mm, (xr, "win"), (pb,))
                            sc = o.get("scale", 1.0)
                            if g % 2 == 0:
                                dve(lambda e, st=st, bank=bank, T=T, sc=sc: e.tensor_scalar(
                                    out=st[:, 0, 0:T], in0=ps[:, bank, 0:T], scalar1=sc, scalar2=None, op0=ALU.mult), (pb,), (sr,))
                            else:
                                act(lambda e, st=st, bank=bank, T=T, sc=sc: e.activation(
                                    out=st[:, 0, 0:T], in_=ps[:, bank, 0:T], func=AF.Copy, scale=sc), (pb,), (sr,))
                            dma("sp", sr, o["dst"](ft, t0, T), st[:, 0, 0:T], (sr,), (o["res"],))
                    else:
                        for sub in range(T // 128):
                            g = cnt["grp"]; cnt["grp"] += 1
                            bank = g % 8
                            pb = "ps%d" % bank
                            st = stgs[g % NSTG]; sr = "stg%d" % (g % NSTG)

                            def mm(e, xt=xt, bank=bank, sub=sub, w0=o["w0"], s0=s0):
                                for c in range(8):
                                    i = e.matmul(ps[:, bank, :], lhsT=xt[:, c, s0 + sub * 128:s0 + (sub + 1) * 128],
                                                 rhs=win[:, c, w0:w0 + 512], start=(c == 0), stop=(c == 7))
                                return i
                            pe(mm, (xr, "win"), (pb,))
                            if g % 2 == 0:
                                dve(lambda e, st=st, bank=bank: e.tensor_copy(out=st[:, 0, :], in_=ps[:, bank, :]), (pb,), (sr,))
                            else:
                                act(lambda e, st=st, bank=bank: e.activation(out=st[:, 0, :], in_=ps[:, bank, :], func=AF.Copy), (pb,), (sr,))
                            dma("sp", sr, o["dst"](t0 + sub * 128), st[:, 0, :], (sr,), (o["res"],))

    proj_stream(xT_all, S, [
        dict(kind="fm", w0=2048, nf=512, tmax=S, res="KT_B", dst=lambda ft, t0, T: KT_B[ft, :, t0:t0 + T]),
        dict(kind="tm", w0=2560, tmax=S, res="V_B", dst=lambda tok: V_B[tok:tok + 128, :]),
        dict(kind="fm", w0=1536, nf=512, tmax=OWN, scale=0.125, res="QT_B", dst=lambda ft, t0, T: QT_B[ft, :, t0:t0 + T]),
    ])
    proj_stream(xTa, LAT, [
        dict(kind="fm", w0=0, nf=512, tmax=LAT, scale=0.125, res="QT_A", dst=lambda ft, t0, T: QT_A[ft * 128:(ft + 1) * 128, t0:t0 + T]),
        dict(kind="fm", w0=512, nf=512, tmax=LAT, res="KT_A", dst=lambda ft, t0, T: KT_A[ft * 128:(ft + 1) * 128, t0:t0 + T]),
        dict(kind="tm", w0=1024, tmax=LAT, res="V_A", dst=lambda tok: V_A[tok:tok + 128, :]),
    ])
    P.barrier()

    biasA = view(0, [12, 512], BF16)
    qas = [view(12288 + 12288 * i, [1, 6144], BF16) for i in range(2)]
    kas = [view(36864 + 12288 * i, [1, 6144], BF16) for i in range(2)]
    vas = [view(61440 + 6272 * i, [48, 65], BF16) for i in range(2)]
    accO = view(73984, [1, 4096], F32)
    pTa = [view(90368 + 1024 * i, [1, 512], BF16) for i in range(2)]
    rl = view(92416, [1, 4096], F32)
    mst = [view(108800 + 1024 * i, [1, 512], BF16) for i in range(2)]
    dma("pool", "biasA", biasA[:, :, :], biasA_in.rearrange("p (a b) -> p a b", b=512), (), ("biasA",))
    for i in range(2):
        dve(lambda e, i=i: e.memset(qas[i][64:65, 0, :], 1.0), (), ("qa1_%d" % i,))
        dve(lambda e, i=i: e.memset(vas[i][:, :, 64:65], 1.0), (), ("vao%d" % i,))

    def a_loads(h, p, sl):
        L = LA[p]; off = LA_OFF[p]
        qa, ka, va = qas[sl], kas[sl], vas[sl]
        qr, kr, vr = "qa%d" % sl, "ka%d" % sl, "va%d" % sl
        dma("sp", qr, qa[0:64, 0, 0:L], QT_A[h * 64:(h + 1) * 64, off:off + L], ("QT_A",), (qr,))
        dma("sp", kr, ka[0:64, 0, 0:L], KT_A[h * 64:(h + 1) * 64, off:off + L], ("KT_A",), (kr,))
        dma("pool", kr + "v", ka[64:65, 0, 0:L], kvalid[0:1, off:off + L], (), (kr + "v",))
        nb = L // 128
        for b0 in range(0, nb, 16):
            b1 = min(nb, b0 + 16)
            dma("sp", vr, va[:, b0:b1, 0:64],
                V_A[off + b0 * 128:off + b1 * 128, h * 64:(h + 1) * 64].rearrange("(b p) d -> p b d", p=128), ("V_A",), (vr,))

    units = []
    for h in range(8):
        for p in range(3):
            for b in range(16):
                units.append((h, p, b))
    a_loads(0, 0, 0)

    def a_qk(u):
        h, p, b = units[u]
        sl = (h * 3 + p) % 2
        L = LA[p]; dil = DIL[p]
        qa, ka = qas[sl], kas[sl]
        per = 16 // dil
        r = b // per; bb = b % per
        s0 = r * (L // dil) + 256 * bb
        sb = 4 + (u % 2)
        bi = 2 * p - h - 1 + 8

        def qk(e):
            e.matmul(ps[:, sb, :], lhsT=ident_b[:, :], rhs=biasA[:, bi, :], start=True, stop=False)
            e.matmul(ps[:, sb, 0:128], lhsT=ka[0:65, 0, s0:s0 + 128], rhs=qa[0:65, 0, s0 + 64:s0 + 192], start=False, stop=False)
            e.matmul(ps[:, sb, 128:384], lhsT=ka[0:65, 0, s0 + 128:s0 + 256], rhs=qa[0:65, 0, s0 + 64:s0 + 320], start=False, stop=False)
            return e.matmul(ps[:, sb, 384:512], lhsT=ka[0:65, 0, s0 + 256:s0 + 384], rhs=qa[0:65, 0, s0 + 192:s0 + 320], start=False, stop=True)
        pe(qk, ("qa%d" % sl, "qa1_%d" % sl, "ka%d" % sl, "ka%dv" % sl, "biasA", "ident_b"), ("ps%d" % sb,))

    def a_rest(u):
        h, p, b = units[u]
        sl = (h * 3 + p) % 2
        L = LA[p]; dil = DIL[p]
        va = vas[sl]
        per = 16 // dil
        r = b // per; bb = b % per
        s0 = r * (L // dil) + 256 * bb
        sb = 4 + (u % 2); ob = 6 + (u % 2); pt = pTa[u % 2]; ptr = "pTa%d" % (u % 2)
        kb0 = s0 // 128
        act(lambda e: e.activation(out=pt[:, 0, :], in_=ps[:, sb, :], func=AF.Exp), ("ps%d" % sb,), (ptr,))

        def pv(e):
            e.matmul(ps[0:65, ob, 0:256], lhsT=va[:, kb0 + 1, :], rhs=pt[:, 0, 128:384], start=True, stop=False)
            e.matmul(ps[0:65, ob, 0:128], lhsT=va[:, kb0, :], rhs=pt[:, 0, 0:128], start=False, stop=False)
            return e.matmul(ps[0:65, ob, 128:256], lhsT=va[:, kb0 + 2, :], rhs=pt[:, 0, 384:512], start=False, stop=True)
        return pv, ("va%d" % sl, "vao%d" % sl, ptr), ("ps%d" % ob,), ob, r + dil * 256 * bb, dil, p

    def a_norm(h):
        act(lambda e: e.activation(out=rl[64:65, 0, :], in_=accO[64:65, 0, :], func=AF.Ln), ("accO",), ("rl",))
        act(lambda e: e.activation(out=rl[64:65, 0, :], in_=rl[64:65, 0, :], func=AF.Exp, scale=-1.0), ("rl",), ("rl",))
        for j in range(8):
            bank = j % 4
            pe(lambda e, j=j, bank=bank: e.matmul(ps[:, bank, :], lhsT=ones_f[64:65, :], rhs=rl[64:65, 0, j * 512:(j + 1) * 512],
                                                  start=True, stop=True), ("rl", "ones_f"), ("ps%d" % bank,))
            ms = mst[j % 2]; msr = "mst%d" % (j % 2)
            dve(lambda e, j=j, bank=bank, ms=ms: e.tensor_tensor(out=ms[0:64, 0, :], in0=accO[0:64, 0, j * 512:(j + 1) * 512],
                                                                 in1=ps[0:64, bank, :], op=ALU.mult), ("accO", "ps%d" % bank), (msr,))
            dma("sp", msr, MIXT[h * 64:(h + 1) * 64, j * 512:(j + 1) * 512], ms[0:64, 0, :], (msr,), ("MIXT",))

    a_qk(0)
    for u in range(len(units)):
        h, p, b = units[u]
        if b == 0 and u + 16 < len(units):
            hn, pn, _ = units[u + 16]
            a_loads(hn, pn, (hn * 3 + pn) % 2)
        pvfn, pr, pw, ob, tstart, dil, p = a_rest(u)
        if u + 1 < len(units):
            a_qk(u + 1)
        pe(pvfn, pr, pw)
        dst = accO[0:65, 0, tstart:tstart + dil * 255 + 1:dil]
        if p == 0:
            dve(lambda e, dst=dst, ob=ob: e.tensor_copy(out=dst, in_=ps[0:65, ob, 0:256]), ("ps%d" % ob,), ("accO",))
        else:
            dve(lambda e, dst=dst, ob=ob: e.tensor_tensor(out=dst, in0=ps[0:65, ob, 0:256], in1=dst, op=ALU.add),
                ("ps%d" % ob, "accO"), ("accO",))
        if p == 2 and b == 15:
            a_norm(h)
    P.barrier()

    KTs = [view(32768 * c, [1, S], BF16) for c in range(2)]
    V1 = view(65536, [128, 129], BF16)
    QTv = [view(98560 + 6144 * i, [6, 512], BF16) for i in range(2)]
    pTb = [view(110848 + 2048 * i, [2, 512], BF16) for i in range(2)]
    accS = view(114944, [8, 129], F32)
    diagB = view(119072, [4, 128], BF16)
    odt = view(120096, [4, 128], F32)
    t1 = view(122144, [1, 128], F32)
    odn = view(122656, [4, 128], BF16)
    sq = view(123680, [1, 128], F32)
    mixst = [view(124192 + 1024 * i, [1, 512], BF16) for i in range(2)]
    stat = small("statB", [32])
    dma("pool", "diagB", diagB[:, :, :], diagB_in.rearrange("p (a b) -> p a b", b=128), (), ("diagB",))
    for c in range(2):
        dma("pool", "kaug%d" % c, KTs[c][64:68, 0, :], kaugB[:, :], (), ("KTa%d" % c,))
    dve(lambda e: e.memset(V1[:, :, 128:129], 1.0), (), ("V1o",))
    ACC_POS = [(0, 0), (0, 129), (0, 258), (1, 0), (1, 129), (1, 258), (2, 0), (2, 129)]

    def b_kvloads(h):
        for q in range(4):
            kv = "KVq%d" % q
            for c in range(2):
                dma("sp", kv, KTs[c][0:64, 0, q * 4096:(q + 1) * 4096],
                    KT_B[h, c * 64:(c + 1) * 64, q * 4096:(q + 1) * 4096], ("KT_B",), (kv,))
            for b0 in (q * 32, q * 32 + 16):
                dma("sp", kv, V1[:, b0:b0 + 16, 0:128],
                    V_B[b0 * 128:(b0 + 16) * 128, h * 128:(h + 1) * 128].rearrange("(b p) d -> p b d", p=128), ("V_B",), (kv,))

    def b_qloads(g):
        h, qb = g // 8, g % 8
        sl = g % 2
        qt = QTv[sl]; qtr = "QTv%d" % sl
        for var in range(3):
            for c in range(2):
                dma("sp", qtr, qt[0:64, var * 2 + c, :], QT_B[h, c * 64:(c + 1) * 64, qb * 512:(qb + 1) * 512], ("QT_B",), (qtr,))
        dma("pool", qtr + "a", qt[64:68, :, :], qaugB[h, qb].rearrange("r (v t) -> r v t", t=512), (), (qtr + "a",))

    def b_qk(g, kb, u):
        h, qb = g // 8, g % 8
        st_ = u % 2
        qt = QTv[g % 2]; qtr = "QTv%d" % (g % 2)

        def qk(e):
            i = None
            for c in range(2):
                o = ps[:, 2 * st_ + c, :]
                kt = KTs[c][0:68, 0, kb * 128:(kb + 1) * 128]
                if kb >= 32 or kb // 4 != qb:
                    var = 0 if (kb >= 32 or kb // 4 > qb) else 1
                    i = e.matmul(o, lhsT=kt, rhs=qt[0:68, var * 2 + c, :], start=True, stop=True)
                else:
                    j = kb % 4
                    if j > 0:
                        e.matmul(o[:, 0:128 * j], lhsT=kt, rhs=qt[0:68, 0 + c, 0:128 * j], start=True, stop=True)
                    if j < 3:
                        e.matmul(o[:, 128 * (j + 1):512], lhsT=kt, rhs=qt[0:68, 2 + c, 128 * (j + 1):512], start=True, stop=True)
                    e.matmul(o[:, 128 * j:128 * (j + 1)], lhsT=ident_b[:, :], rhs=diagB[:, h, :], start=True, stop=False)
                    i = e.matmul(o[:, 128 * j:128 * (j + 1)], lhsT=kt, rhs=qt[0:68, 4 + c, 128 * j:128 * (j + 1)],
                                 start=False, stop=True)
            return i
        pe(qk, ("KVq%d" % (kb // 32), "KTa0", "KTa1", qtr, qtr + "a", "diagB", "ident_b"), ("psS%d" % st_,))

    def b_post(g):
        h, qb = g // 8, g % 8
        for a in range(8):
            bk, col = ACC_POS[a]
            if a % 2 == 0:
                dve(lambda e, a=a, bk=bk, col=col: e.tensor_copy(out=accS[:, a, :], in_=ps[:, 4 + bk, col:col + 129]), ("accP",), ("accS",))
            else:
                act(lambda e, a=a, bk=bk, col=col: e.activation(out=accS[:, a, :], in_=ps[:, 4 + bk, col:col + 129], func=AF.Copy), ("accP",), ("accS",))
        dve(lambda e: e.reciprocal(out=stat[:, 0:8], in_=accS[:, :, 128]), ("accS",), ("stat",))
        dve(lambda e: e.tensor_scalar(out=stat[:, 8:12], in0=stat[:, 4:8], scalar1=nlam[:, 0:1], scalar2=None, op0=ALU.mult), ("stat", "nlam"), ("stat",))
        for qs in range(4):
            dve(lambda e, qs=qs: e.tensor_scalar(out=t1[:, 0, :], in0=accS[:, qs, 0:128], scalar1=stat[:, qs:qs + 1], scalar2=None, op0=ALU.mult),
                ("accS", "stat"), ("t1",))
            dve(lambda e, qs=qs: e.scalar_tensor_tensor(out=odt[:, qs, :], in0=accS[:, 4 + qs, 0:128], scalar=stat[:, 8 + qs:9 + qs], in1=t1[:, 0, :],
                                                        op0=ALU.mult, op1=ALU.add), ("accS", "stat", "t1"), ("odt",))
            dve(lambda e, qs=qs: e.scalar_tensor_tensor(out=sq[:, 0, :], in0=odt[:, qs, :], scalar=1.0, in1=odt[:, qs, :], op0=ALU.mult, op1=ALU.mult,
                                                        accum_out=stat[:, 12 + qs:13 + qs]), ("odt",), ("sq", "stat"))
        act(lambda e: e.activation(out=stat[:, 16:20], in_=stat[:, 12:16], func=AF.Sqrt, bias=EPS, scale=1.0 / 128.0), ("stat",), ("stat",))
        dve(lambda e: e.reciprocal(out=stat[:, 20:24], in_=stat[:, 16:20]), ("stat",), ("stat",))
        ms = mixst[g % 2]; msr = "mixst%d" % (g % 2)
        for qs in range(4):
            dve(lambda e, qs=qs: e.scalar_tensor_tensor(out=odn[:, qs, :], in0=odt[:, qs, :], scalar=stat[:, 20 + qs:21 + qs], in1=gsc[:, :],
                                                        op0=ALU.mult, op1=ALU.mult), ("odt", "stat", "gsc"), ("odn",))
        tb = ps[:, 7, :].bitcast(BF16)

        def tp(e):
            i = None
            for qs in range(4):
                i = e.transpose(out=tb[:, qs * 128:(qs + 1) * 128], in_=odn[:, qs, :], identity=ident_b[:, :])
            return i
        pe(tp, ("odn", "ident_b"), ("ps7",))
        dve(lambda e, ms=ms: e.tensor_copy(out=ms[:, 0, :], in_=tb[:, 0:512]), ("ps7",), (msr,))
        dma("sp", msr, MIXT[512 + h * 128:512 + (h + 1) * 128, qb * 512:(qb + 1) * 512], ms[:, 0, :], (msr,), ("MIXT",))

    NG = 32

    def b_needed(h, qb, kb):
        dmax = 150.0 * (2.0 ** (2 * (h + 1)))
        q0, q1 = qb * 512, qb * 512 + 511
        k0, k1 = kb * 128, kb * 128 + 127
        if kb < 32:
            gap = max(0, k0 - q1, q0 - k1)
        else:
            gap = min(k0 - q1, q0 - (k1 - S))
        return gap <= dmax

    glist = [[kb for kb in range(128) if b_needed(g // 8, g % 8, kb)] for g in range(NG)]
    flat = [(g, i) for g in range(NG) for i in range(len(glist[g]))]
    b_kvloads(0)
    b_qloads(0)
    b_qk(0, glist[0][0], 0)
    for u, (g, i) in enumerate(flat):
        h, qb = g // 8, g % 8
        kb = glist[g][i]
        if i == 0 and g + 1 < NG:
            b_qloads(g + 1)
        st_ = u % 2
        pt = pTb[st_]; ptr = "pTb%d" % st_
        act(lambda e, pt=pt, st_=st_: e.activation(out=pt[:, :, :], in_=ps[:, 2 * st_:2 * st_ + 2, :], func=AF.Exp), ("psS%d" % st_,), (ptr,))
        if u + 1 < len(flat):
            gn, i_n = flat[u + 1]
            if gn != g and gn % 8 == 0:
                b_kvloads(gn // 8)
            b_qk(gn, glist[gn][i_n], u + 1)
        first = (i == 0); last = (i == len(glist[g]) - 1)

        def pv(e, kb=kb, pt=pt, first=first, last=last):
            ins = None
            for a in range(8):
                c, qs = a // 4, a % 4
                bk, col = ACC_POS[a]
                ins = e.matmul(ps[:, 4 + bk, col:col + 129], lhsT=pt[:, c, qs * 128:(qs + 1) * 128], rhs=V1[:, kb, :],
                               start=(first and col == 0), stop=last, skip_group_check=True)
            return ins
        pe(pv, (ptr, "KVq%d" % (kb // 32), "V1o"), ("accP",))
        if last:
            b_post(g)
    P.barrier()

    woA = view(0, [4, 1024], BF16)
    woB = view(16384, [4, 1024], BF16)
    mixA = [view(24576 + 8192 * i, [4, 512], BF16) for i in range(2)]
    mixB = [view(40960 + 4096 * i, [4, 512], BF16) for i in range(2)]
    xo = [view(49152 + 4096 * i, [1, 1024], F32) for i in range(2)]
    h1 = [view(57344 + 4096 * i, [1, 1024], F32) for i in range(2)]
    x1t = [view(65536 + 4096 * i, [1, 1024], F32) for i in range(2)]
    x1b = [view(73728 + 2048 * i, [1, 1024], BF16) for i in range(2)]
    x1T = [view(77824 + 4096 * i, [8, 128], F32) for i in range(2)]
    lng = view(86016, [1, 1024], F32); lnb = view(90112, [1, 1024], F32)
    wr = view(94208, [8, 32], F32)
    sm = [[view(95232 + 1024 * i + 128 * k, [1, 32], F32) for k in range(6)] for i in range(2)]
    mskb = [view(97280 + 64 * i, [1, 32], BF16) for i in range(2)]
    top8 = [small("top8_%d" % i, [8]) for i in range(2)]
    bst = [small("bst%d" % i, [12]) for i in range(2)]
    mv = [small("mv%d" % i, [2]) for i in range(2)]
    st2 = [small("st2_%d" % i, [4]) for i in range(2)]
    dma("pool", "woA", woA[:, :, :], w_out[0:512, :].rearrange("(g p) f -> p g f", p=128), (), ("woA",))
    dma("pool", "woB", woB[:, :, :], w_out[512:1024, :].rearrange("(h d) f -> d h f", d=128), (), ("woB",))
    dma("sp", "lng", lng[:, 0, :], ln1_g.broadcast_to([128, D]), (), ("lng",))
    dma("sp", "lnb", lnb[:, 0, :], ln1_b.broadcast_to([128, D]), (), ("lnb",))
    dma("sp", "wr", wr[:, :, :], w_router.rearrange("(c p) e -> p c e", p=128), (), ("wr",))

    def layer_norm(src, dst, g_ap, b_ap, tag, res_src, res_dst, i2, on_act=False):
        b_, m_, s_ = bst[i2], mv[i2], st2[i2]
        bn, mn, sn = "bst%d" % i2, "mv%d" % i2, "st2_%d" % i2
        for i in range(2):
            dve(lambda e, i=i: e.bn_stats(out=b_[:, 6 * i:6 * (i + 1)], in_=src[:, 0, 512 * i:512 * (i + 1)]), (res_src,), (bn,))
        dve(lambda e: e.bn_aggr(out=m_[:, :], in_=b_[:, :]), (bn,), (mn,))
        act(lambda e: e.activation(out=s_[:, 0:1], in_=m_[:, 1:2], func=AF.Sqrt, bias=EPS, scale=1.0), (mn,), (sn,))
        dve(lambda e: e.reciprocal(out=s_[:, 1:2], in_=s_[:, 0:1]), (sn,), (sn,))
        dve(lambda e: e.tensor_scalar(out=dst[:, 0, :], in0=src[:, 0, :], scalar1=m_[:, 0:1], scalar2=s_[:, 1:2], op0=ALU.subtract, op1=ALU.mult),
            (res_src, mn, sn), (res_dst,))
        dve(lambda e: e.tensor_tensor(out=dst[:, 0, :], in0=dst[:, 0, :], in1=g_ap, op=ALU.mult), (res_dst, tag + "g"), (res_dst,))
        dve(lambda e: e.tensor_tensor(out=dst[:, 0, :], in0=dst[:, 0, :], in1=b_ap, op=ALU.add), (res_dst, tag + "b"), (res_dst,))

    def c_loads(tt):
        s2 = tt % 2
        if tt % 4 == 0:
            ms2 = (tt // 4) % 2
            dma("sp", "mixA%d" % ms2, mixA[ms2][:, :, :], MIXT[0:512, tt * 128:tt * 128 + 512].rearrange("(g p) t -> p g t", p=128), ("MIXT",), ("mixA%d" % ms2,))
            dma("sp", "mixB%d" % ms2, mixB[ms2][:, :, :], MIXT[512:1024, tt * 128:tt * 128 + 512].rearrange("(h d) t -> d h t", d=128), ("MIXT",), ("mixB%d" % ms2,))
        dma("sp", "xo%d" % s2, xo[s2][:, 0, :], x_own[tt * 128:(tt + 1) * 128, :], (), ("xo%d" % s2,))

    def c_stageA1(tt):
        s2 = tt % 2
        lg, msk, ex, gg, posf, junk = sm[s2]
        R = lambda n: "%s%d" % (n, s2)
        ms2 = (tt // 4) % 2
        tq = (tt % 4) * 128
        lg, msk, ex, gg, posf, junk = sm[s2]
        R = lambda n: "%s%d" % (n, s2)
        yb = 2 * s2

        def mmo(e, ms2=ms2, tq=tq, yb=yb):
            i = None
            for n in range(2):
                for hh in range(4):
                    i = e.matmul(ps[:, yb + n, :], lhsT=mixA[ms2][:, hh, tq:tq + 128], rhs=woA[:, hh, n * 512:(n + 1) * 512], start=(hh == 0), stop=False)
                for hh in range(4):
                    i = e.matmul(ps[:, yb + n, :], lhsT=mixB[ms2][:, hh, tq:tq + 128], rhs=woB[:, hh, n * 512:(n + 1) * 512], start=False, stop=(hh == 3))
            return i
        pe(mmo, ("mixA%d" % ms2, "mixB%d" % ms2, "woA", "woB"), (R("psY"),))
        dve(lambda e, s2=s2, yb=yb: e.scalar_tensor_tensor(out=h1[s2][:, 0, :], in0=xo[s2][:, 0, :], scalar=ALPHA, in1=ps[:, yb:yb + 2, :].rearrange("p a b -> p (a b)"),
                                                           op0=ALU.mult, op1=ALU.add), (R("xo"), R("psY")), (R("h1"),))

    def c_stageA2(tt):
        s2 = tt % 2
        R = lambda n: "%s%d" % (n, s2)
        layer_norm(h1[s2], x1t[s2], lng[:, 0, :], lnb[:, 0, :], "ln", R("h1"), R("x1t"), s2)
        dma("sp", R("x1w"), X1[tt * 128:(tt + 1) * 128, :], x1t[s2][:, 0, :], (R("x1t"),), ("X1",))
        act(lambda e, s2=s2: e.activation(out=x1b[s2][:, 0, :], in_=x1t[s2][:, 0, :], func=AF.Copy), (R("x1t"),), (R("x1b"),))

        def tpx(e, s2=s2):
            i = None
            for c in range(8):
                i = e.transpose(out=ps[:, 4 + c // 4, (c % 4) * 128:(c % 4 + 1) * 128], in_=x1t[s2][:, 0, c * 128:(c + 1) * 128], identity=ident_f[:, :])
            return i
        pe(tpx, (R("x1t"), "ident_f"), ("psT",))
        act(lambda e, s2=s2: e.activation(out=x1T[s2][:, :, :], in_=ps[:, 4:6, :].rearrange("p b (c s) -> p (b c) s", s=128), func=AF.Copy), ("psT",), (R("x1T"),))

    def c_stageB(tt):
        s2 = tt % 2
        lg, msk, ex, gg, posf, junk = sm[s2]
        R = lambda n: "%s%d" % (n, s2)
        lb = 6 + s2

        def mmr(e, s2=s2, lb=lb):
            i = None
            for c in range(8):
                i = e.matmul(ps[:, lb, 0:32], lhsT=x1T[s2][:, c, :], rhs=wr[:, c, :], start=(c == 0), stop=(c == 7))
            return i
        pe(mmr, (R("x1T"), "wr"), (R("psL"),))
        t8 = top8[s2]; s_ = st2[s2]; mb = mskb[s2]
        dve(lambda e, lg=lg, lb=lb: e.tensor_tensor(out=lg[:, 0, :], in0=ps[:, lb, 0:32], in1=brt[:, :], op=ALU.add), (R("psL"), "brt"), (R("lg"),))
        dve(lambda e, lg=lg, t8=t8: e.max(out=t8[:, :], in_=lg[:, 0, :]), (R("lg"),), (R("top8"),))
        dve(lambda e, lg=lg, t8=t8, msk=msk: e.tensor_scalar(out=msk[:, 0, :], in0=lg[:, 0, :], scalar1=t8[:, 3:4], scalar2=None, op0=ALU.is_ge), (R("lg"), R("top8")), (R("msk"),))
        dve(lambda e, msk=msk, mb=mb: e.tensor_copy(out=mb[:, 0, :], in_=msk[:, 0, :]), (R("msk"),), (R("mskb"),))
        dve(lambda e, t8=t8, s_=s_: e.tensor_scalar(out=s_[:, 2:3], in0=t8[:, 0:1], scalar1=-1.0, scalar2=None, op0=ALU.mult), (R("top8"),), (R("st2n"),))
        act(lambda e, lg=lg, ex=ex, s_=s_: e.activation(out=ex[:, 0, :], in_=lg[:, 0, :], func=AF.Exp, bias=s_[:, 2:3], scale=1.0), (R("lg"), R("st2n")), (R("ex"),))
        dve(lambda e, ex=ex, msk=msk, gg=gg, s_=s_: e.scalar_tensor_tensor(out=gg[:, 0, :], in0=ex[:, 0, :], scalar=1.0, in1=msk[:, 0, :], op0=ALU.mult, op1=ALU.mult,
                                                                         accum_out=s_[:, 3:4]), (R("ex"), R("msk")), (R("gg"), R("st2s")))
        dve(lambda e, s_=s_: e.reciprocal(out=s_[:, 3:4], in_=s_[:, 3:4]), (R("st2s"),), (R("st2s"),))
        dve(lambda e, gg=gg, s_=s_: e.tensor_scalar(out=gg[:, 0, :], in0=gg[:, 0, :], scalar1=s_[:, 3:4], scalar2=None, op0=ALU.mult), (R("gg"), R("st2s")), (R("gg"),))

        def mmk(e, mb=mb, lb=lb):
            e.matmul(ps[:, lb, 32:64], lhsT=ltri_b[:, :], rhs=mb[:, 0, :], start=True, stop=False)
            return e.matmul(ps[:, lb, 32:64], lhsT=ones_b[:, :], rhs=cum_b[:, :], start=False, stop=True)
        pe(mmk, (R("mskb"), "cum_b", "ltri_b", "ones_b", R("lg")), (R("psK"),))
        dve(lambda e, posf=posf, lb=lb: e.tensor_scalar(out=posf[:, 0, :], in0=ps[:, lb, 32:64], scalar1=float(CAP - 1), scalar2=None, op0=ALU.min), (R("psK"),), (R("posf"),))
        dve(lambda e, posf=posf: e.tensor_tensor(out=posf[:, 0, :], in0=posf[:, 0, :], in1=eoff[:, :], op=ALU.add), (R("posf"), "eoff"), (R("posf"),))
        dve(lambda e, msk=msk: e.tensor_tensor(out=cum_f[:, :], in0=cum_f[:, :], in1=msk[:, 0, :], op=ALU.add), ("cum_f", R("msk")), ("cum_f",))
        dve(lambda e: e.tensor_copy(out=cum_b[:, :], in_=cum_f[:, :]), ("cum_f",), ("cum_b",))
        for k in range(4):
            dve(lambda e, k=k, tt=tt, lg=lg, t8=t8, posf=posf, junk=junk: e.scalar_tensor_tensor(
                out=junk[:, 0, :], in0=lg[:, 0, :], scalar=t8[:, k:k + 1], in1=posf[:, 0, :],
                op0=ALU.is_equal, op1=ALU.mult, accum_out=sl_f[:, tt * 4 + k:tt * 4 + k + 1]),
                (R("lg"), R("top8"), R("posf")), (R("junk"), "sl_f%d" % tt))
            dve(lambda e, k=k, tt=tt, lg=lg, t8=t8, gg=gg, junk=junk: e.scalar_tensor_tensor(
                out=junk[:, 0, :], in0=lg[:, 0, :], scalar=t8[:, k:k + 1], in1=gg[:, 0, :],
                op0=ALU.is_equal, op1=ALU.mult, accum_out=gatek[:, tt * 4 + k:tt * 4 + k + 1]),
                (R("lg"), R("top8"), R("gg")), (R("junk"), "gatek%d" % tt))
        dve(lambda e, tt=tt: e.tensor_copy(out=sl_i[:, tt * 4:tt * 4 + 4], in_=sl_f[:, tt * 4:tt * 4 + 4]), ("sl_f%d" % tt,), ("sl_i%d" % tt,))
        for k in range(4):
            P.add("pool", lambda e, k=k, tt=tt, s2=s2: e.indirect_dma_start(
                out=XE[:, :], out_offset=bass.IndirectOffsetOnAxis(ap=sl_i[:, tt * 4 + k:tt * 4 + k + 1], axis=0),
                in_=x1b[s2][:, 0, :], in_offset=None), (R("x1b"), "sl_i%d" % tt), ("XE",), chan="sc%d" % s2)

    c_loads(0)
    c_loads(1)
    c_stageA1(0)
    c_loads(2)
    c_stageA1(1)
    c_stageA2(0)
    for tt in range(NT):
        if tt + 3 < NT:
            c_loads(tt + 3)
        if tt + 2 < NT:
            c_stageA1(tt + 2)
        if tt + 1 < NT:
            c_stageA2(tt + 1)
        c_stageB(tt)
    P.barrier()

    wu = [view(32768 * i, [8, 2048], BF16) for i in range(2)]
    wd = [view(65536 + 16384 * i, [8, 1024], BF16) for i in range(2)]
    NST = CAP // 128
    xe = [view(98304, [NST, 1024], BF16)] * 2
    xeT = [view(110592 + 12288 * i, [8, CAP], BF16) for i in range(2)]
    aT = [view(135168 + 12288 * i, [8, CAP], BF16) for i in range(2)]
    tmp = [view(159744 + 1536 * i, [1, CAP // 2], F32) for i in range(8)]
    ysb = [view(172032 + 4096 * i, [1, 1024], F32) for i in range(2)]
    bdn = [view(180224 + 4096 * i, [1, 1024], F32) for i in range(2)]
    bup = view(188416, [NE, 16], F32)
    dma("sp", "bup", bup[:, :, :], b_upT.rearrange("p (e c) -> p e c", c=16), (), ("bup",))
    HALF = CAP // 2
    ectr = {"gi": 0, "yi": 0}

    def e_load(ex_):
        s2 = ex_ % 2
        for c in range(8):
            dma("pool", "wu%d" % s2, wu[s2][:, c, :], w_up[ex_, c * 128:(c + 1) * 128, :], (), ("wu%d" % s2,))
        for c in range(8):
            dma("pool", "wd%d" % s2, wd[s2][:, c, :], w_down[ex_, c * 128:(c + 1) * 128, :], (), ("wd%d" % s2,))
        dma("sp", "bdn%d" % s2, bdn[s2][:, 0, :], b_down[ex_:ex_ + 1, :].broadcast_to([128, D]), (), ("bdn%d" % s2,))

    def e_load_x(ex_):
        s2 = ex_ % 2
        dma("sp", "xe", xe[s2][:, :, :], XE[ex_ * CAP:(ex_ + 1) * CAP, :].rearrange("(s p) d -> p s d", p=128), ("XE",), ("xe",))

    def e_T(ex_):
        s2 = ex_ % 2
        for st_ in range(NST):
            for cg in range(2):
                bank = (st_ * 2 + cg) % 2
                tb = ps[:, bank, :].bitcast(BF16)

                def tpe(e, st_=st_, cg=cg, tb=tb):
                    i = None
                    for cc in range(4):
                        c = cg * 4 + cc
                        i = e.transpose(out=tb[:, cc * 128:(cc + 1) * 128], in_=xe[s2][:, st_, c * 128:(c + 1) * 128], identity=ident_b[:, :])
                    return i
                pe(tpe, ("xe", "ident_b"), ("psE%d" % bank,))
                if (st_ + cg) % 2 == 0:
                    dve(lambda e, st_=st_, cg=cg, tb=tb: e.tensor_copy(
                        out=xeT[s2][:, cg * 4:cg * 4 + 4, st_ * 128:(st_ + 1) * 128], in_=tb[:, 0:512].rearrange("p (c s) -> p c s", s=128)),
                        ("psE%d" % bank,), ("xeT%d" % s2,))
                else:
                    act(lambda e, st_=st_, cg=cg, tb=tb: e.activation(
                        out=xeT[s2][:, cg * 4:cg * 4 + 4, st_ * 128:(st_ + 1) * 128], in_=tb[:, 0:512].rearrange("p (c s) -> p c s", s=128), func=AF.Copy),
                        ("psE%d" % bank,), ("xeT%d" % s2,))

    def e_U(ex_):
        s2 = ex_ % 2
        for j in range(8):
            for hf in range(2):
                gs = ectr["gi"] % 2; ectr["gi"] += 1
                bg, bu = 2 + 2 * gs, 3 + 2 * gs
                c0 = hf * HALF

                def up(e, j=j, c0=c0, bg=bg, bu=bu):
                    i = None
                    for (bank, fc) in ((bg, j), (bu, j + 8)):
                        for c in range(8):
                            i = e.matmul(ps[:, bank, 0:HALF], lhsT=wu[s2][:, c, fc * 128:(fc + 1) * 128], rhs=xeT[s2][:, c, c0:c0 + HALF],
                                         start=(c == 0), stop=(c == 7))
                    return i
                pe(up, ("wu%d" % s2, "xeT%d" % s2), ("psU%d" % gs,))
                gl, sg, ul, a1 = tmp[4 * gs], tmp[4 * gs + 1], tmp[4 * gs + 2], tmp[4 * gs + 3]
                tg = "tmp%d" % gs
                dve(lambda e, gl=gl, bg=bg, j=j: e.tensor_scalar(out=gl[:, 0, :], in0=ps[:, bg, 0:HALF], scalar1=bup[:, ex_, j:j + 1], scalar2=7.0,
                                                                 op0=ALU.add, op1=ALU.min), ("psU%d" % gs, "bup"), (tg + "gl",))
                act(lambda e, gl=gl, sg=sg: e.activation(out=sg[:, 0, :], in_=gl[:, 0, :], func=AF.Sigmoid, scale=1.702), (tg + "gl",), (tg + "sg",))
                dve(lambda e, ul=ul, bu=bu, j=j: e.tensor_scalar(out=ul[:, 0, :], in0=ps[:, bu, 0:HALF], scalar1=bup[:, ex_, 8 + j:9 + j], scalar2=7.0,
                                                                 op0=ALU.add, op1=ALU.min), ("psU%d" % gs, "bup"), (tg + "ul",))
                dve(lambda e, ul=ul: e.tensor_scalar(out=ul[:, 0, :], in0=ul[:, 0, :], scalar1=-7.0, scalar2=1.0, op0=ALU.max, op1=ALU.add),
                    (tg + "ul",), (tg + "ul",))
                dve(lambda e, gl=gl, sg=sg, a1=a1: e.tensor_tensor(out=a1[:, 0, :], in0=gl[:, 0, :], in1=sg[:, 0, :], op=ALU.mult),
                    (tg + "gl", tg + "sg"), (tg + "a1",))
                dve(lambda e, a1=a1, ul=ul, j=j, c0=c0: e.tensor_tensor(out=aT[s2][:, j, c0:c0 + HALF], in0=a1[:, 0, :], in1=ul[:, 0, :], op=ALU.mult),
                    (tg + "a1", tg + "ul"), ("aT%d" % s2,))

    def e_D(ex_):
        s2 = ex_ % 2
        for st_ in range(NST):
            ys = ysb[ectr["yi"] % 2]; ysr = "ysb%d" % (ectr["yi"] % 2); ectr["yi"] += 1
            for n in range(2):
                bank = 6 + n

                def dn(e, st_=st_, n=n, bank=bank):
                    i = None
                    for j in range(8):
                        i = e.matmul(ps[:, bank, :], lhsT=aT[s2][:, j, st_ * 128:(st_ + 1) * 128], rhs=wd[s2][:, j, n * 512:(n + 1) * 512],
                                     start=(j == 0), stop=(j == 7))
                    return i
                pe(dn, ("aT%d" % s2, "wd%d" % s2), ("psD%d" % n,))
                dve(lambda e, ys=ys, n=n, bank=bank: e.tensor_tensor(out=ys[:, 0, n * 512:(n + 1) * 512], in0=ps[:, bank, :],
                                                                    in1=bdn[s2][:, 0, n * 512:(n + 1) * 512], op=ALU.add),
                    ("psD%d" % n, "bdn%d" % s2), (ysr,))
            dma("sp", ysr, YS[ex_ * CAP + st_ * 128:ex_ * CAP + (st_ + 1) * 128, :], ys[:, 0, :], (ysr,), ("YS",))

    e_load(0)
    e_load(1)
    e_load_x(0)
    e_T(0)
    e_load_x(1)
    e_U(0)
    for ex_ in range(NE):
        if ex_ + 1 < NE:
            e_T(ex_ + 1)
        if ex_ + 2 < NE:
            e_load_x(ex_ + 2)
        e_D(ex_)
        if ex_ + 2 < NE:
            e_load(ex_ + 2)
        if ex_ + 1 < NE:
            e_U(ex_ + 1)
    P.barrier()

    yk = [view(16384 * i, [4, 1024], F32) for i in range(2)]
    x1r = [view(32768 + 4096 * i, [1, 1024], F32) for i in range(2)]
    acc = [view(40960 + 4096 * i, [1, 1024], F32) for i in range(2)]
    outt = [view(49152 + 4096 * i, [1, 1024], F32) for i in range(2)]
    l2g = view(57344, [1, 1024], F32); l2b = view(61440, [1, 1024], F32)
    dma("sp", "l2g", l2g[:, 0, :], ln2_g.broadcast_to([128, D]), (), ("l2g",))
    dma("sp", "l2b", l2b[:, 0, :], ln2_b.broadcast_to([128, D]), (), ("l2b",))
    out_ops = []

    def f_loads(tt):
        s2 = tt % 2
        for k in range(4):
            P.add("pool", lambda e, k=k, tt=tt, s2=s2: e.indirect_dma_start(
                out=yk[s2][:, k, :], out_offset=None, in_=YS[:, :],
                in_offset=bass.IndirectOffsetOnAxis(ap=sl_i[:, tt * 4 + k:tt * 4 + k + 1], axis=0)),
                ("YS", "sl_i%d" % tt), ("yk%d_%d" % (s2, k),), chan="yk%d_%d" % (s2, k))
        dma("sp", "x1r%d" % s2, x1r[s2][:, 0, :], X1[tt * 128:(tt + 1) * 128, :], ("X1",), ("x1r%d" % s2,))

    f_loads(0)
    for tt in range(NT):
        s2 = tt % 2
        if tt + 1 < NT:
            f_loads(tt + 1)
        a = acc[s2]; ar = "acc%d" % s2
        act(lambda e, a=a, s2=s2: e.activation(out=a[:, 0, :], in_=x1r[s2][:, 0, :], func=AF.Copy, scale=ALPHA), ("x1r%d" % s2,), (ar,))
        for k in range(4):
            dve(lambda e, a=a, s2=s2, tt=tt, k=k: e.scalar_tensor_tensor(out=a[:, 0, :], in0=yk[s2][:, k, :], scalar=gatek[:, tt * 4 + k:tt * 4 + k + 1],
                                                                        in1=a[:, 0, :], op0=ALU.mult, op1=ALU.add),
                ("yk%d_%d" % (s2, k), "gatek%d" % tt, ar), (ar,))
        layer_norm(a, outt[s2], l2g[:, 0, :], l2b[:, 0, :], "l2", ar, "outt%d" % s2, s2)
        out_ops.append(dma("sp", "out%d" % s2, out_d[tt * 128:(tt + 1) * 128, :], outt[s2][:, 0, :], ("outt%d" % s2,), ("out",)))
    fin = P.add("sp", None, ("out",), ())
    fin.raw = set(out_ops)

    P.prepare(nc)
    with nc.Block() as block:
        @block.tensor
        def _(e):
            P.emit_engine("pe", e)

        @block.scalar
        def _(e):
            P.emit_engine("act", e)

        @block.vector
        def _(e):
            P.emit_engine("dve", e)

        @block.gpsimd
        def _(e):
            P.emit_engine("pool", e)

        @block.sync
        def _(e):
            P.emit_engine("sp", e)
    return nc


def _tables(core):
    qr = core % 4
    base = qr * OWN
    pos = []
    pos.append(base - 64 + np.arange(LA[0]))
    for r in range(4):
        pos.append(base + r + 4 * (np.arange(1152) - 64))
    for r in range(16):
        pos.append(base + r + 16 * (np.arange(384) - 64))
    posA = np.concatenate(pos)
    validA = (posA >= 0) & (posA < S)
    kvalid = np.where(validA, 0.0, NEG).astype(np.float32)[None, :]
    posA = np.mod(posA, S)
    kpos = np.mod(base + np.arange(S), S)
    sig = np.ones(S, np.float32)
    other = np.arange(S) >= OWN
    sig[other & (kpos < base)] = -1.0
    kaug = np.stack([sig, sig, sig * (kpos // 128), sig * (kpos % 128)]).astype(np.float32)
    qpos = base + np.arange(OWN)
    qaug = np.zeros((4, 8, 4, 3, 2, 512), np.float32)
    for h in range(4):
        sl = 2.0 ** (-8.0 * (h + 1) / 4)
        for vi, sg in enumerate((1.0, -1.0, 0.0)):
            rows = np.stack([sg * sl * 128.0 * (qpos // 128), sg * sl * (qpos % 128),
                             np.full(OWN, -sg * sl * 128.0), np.full(OWN, -sg * sl)])
            rows = rows.reshape(4, 8, 512).transpose(1, 0, 2)
            qaug[h, :, :, vi, 0, :] = rows
            qaug[h, :, :, vi, 1, :] = rows
    return posA, kvalid, kpos, kaug, qaug.reshape(4, 8, 4, 3 * 2 * 512)


def _const_tables():
    ident = np.eye(128, dtype=np.float32)
    ltri = (np.arange(128)[:, None] < np.arange(128)[None, :]).astype(np.float32)
    ones = np.ones((128, 128), np.float32)
    eoff = np.zeros((128, 128), np.float32)
    eoff[:, 0:32] = (np.arange(32) * CAP)[None, :]
    zer = np.zeros((128, 128), np.float32)
    consts = np.concatenate([ident, ltri, ones, eoff, zer], axis=1)
    i = np.arange(128)
    diag = np.zeros((128, 4, 128), np.float32)
    for h in range(4):
        sl = 2.0 ** (-8.0 * (h + 1) / 4)
        diag[:, h, :] = -sl * np.abs(i[:, None] - i[None, :])
    bias = np.full((128, 12, 512), NEG, np.float32)
    kk = np.arange(128)[:, None]
    for idx in range(12):
        sd = 2.0 ** (idx - 8)
        for (c0, c1, joff, ioff) in ((0, 128, 0, 0), (128, 384, 128, 0), (384, 512, 256, 128)):
            cols = np.arange(c0, c1)[None, :]
            ii = cols - c0 + ioff
            dd = (kk + joff) - 64 - ii
            ok = np.abs(dd) <= 64
            bias[:, idx, c0:c1] = np.where(ok, -sd * np.abs(dd), NEG)
    return consts, diag.reshape(128, 512), bias.reshape(128, 12 * 512)


def _prep_inputs(inputs, cores):
    f = lambda a: np.ascontiguousarray(np.asarray(a, dtype=np.float32))
    x = f(inputs["x"])
    consts, diag, biasA = _const_tables()
    shared = {
        "w_in": f(inputs["w_in"][0]), "w_out": f(inputs["w_out"][0]),
        "w_router": f(inputs["w_router"][0]), "b_router": f(inputs["b_router"]),
        "w_up": f(inputs["w_up"][0]), "w_down": f(inputs["w_down"][0]),
        "b_upT": f(np.asarray(inputs["b_up"][0]).reshape(NE, 16, 128).transpose(2, 0, 1).reshape(128, NE * 16)),
        "b_down": f(inputs["b_down"][0]),
        "ln1_g": f(inputs["ln1_g"]), "ln1_b": f(inputs["ln1_b"]), "ln2_g": f(inputs["ln2_g"]), "ln2_b": f(inputs["ln2_b"]),
        "lam_in": f(np.concatenate([np.asarray(inputs[k][0]) for k in ("lambda_q1", "lambda_k1", "lambda_q2", "lambda_k2")])[None, :]),
        "gsub": f(inputs["diff_norm_g"]),
        "diagB": diag, "biasA": biasA, "consts": consts,
    }
    maps = []
    for core in cores:
        b = core // 4
        base = (core % 4) * OWN
        posA, kvalid, kpos, kaug, qaug = _tables(core)
        xb = x[b]
        m = dict(shared)
        m["xT_all"] = np.ascontiguousarray(xb[kpos].T)
        m["xTa"] = np.ascontiguousarray(xb[posA].T)
        m["x_own"] = np.ascontiguousarray(xb[base:base + OWN])
        m["kvalid"] = kvalid
        m["kaugB"] = kaug
        m["qaugB"] = qaug
        maps.append(m)
    return maps


_NC_CACHE = {}


def kernel(**inputs):
    if "nc" not in _NC_CACHE:
        _NC_CACHE["nc"] = build_program()
    nc = _NC_CACHE["nc"]
    cores = list(range(NCORES))
    maps = _prep_inputs(inputs, cores)
    res = run_bass_kernel_spmd(nc, maps, core_ids=cores)
    out = np.zeros((2, S, D), np.float32)
    for core in cores:
        b = core // 4
        base = (core % 4) * OWN
        out[b, base:base + OWN] = res.results[core]["out"]
    return out
```
